# Optimizing a Trainium2 kernel written in Bass

```python
import jax
import jax.numpy as jnp
from jax import lax
import numpy as np

D_MODEL = 1024
BATCH = 32
SEQ = 2048
DEPTH = 2

CHUNK = 64
MLSTM_HEADS = 8
MLSTM_HEAD_DIM = D_MODEL // MLSTM_HEADS
MLSTM_WIDTH = MLSTM_HEADS * MLSTM_HEAD_DIM
MLSTM_CONV = 4
GATE_SOFT_CAP = 15.0
HGRN_HEADS = 8
HGRN_EXPAND = D_MODEL // HGRN_HEADS
HGRN_HEAD_DIM = D_MODEL // HGRN_HEADS
HGRN_KEY_WIDTH = HGRN_HEADS * HGRN_EXPAND
HGRN_WIDTH = HGRN_HEADS * HGRN_HEAD_DIM
HGRN_BLOCK = 16
D_FF = 2816
FFN_CONV = 3
RMS_EPS = 1e-6
NEG_BIG = -1e30
PROJ_COLS = (2 * MLSTM_WIDTH, MLSTM_WIDTH, MLSTM_WIDTH, MLSTM_HEADS, MLSTM_HEADS,
             HGRN_KEY_WIDTH, HGRN_KEY_WIDTH, HGRN_WIDTH, HGRN_WIDTH, D_MODEL, D_MODEL)
PROJ_WIDTH = sum(PROJ_COLS)
OFF_MLSTM_F = 4 * MLSTM_WIDTH + MLSTM_HEADS

kernel_name = 'hybrid_mlstm_hgrn2_convffn_adaln'


def rms_norm(x, w):
    xf = x.astype(jnp.float32)
    y = xf * lax.rsqrt(jnp.mean(xf * xf, axis=-1, keepdims=True) + RMS_EPS)
    return (y * w.astype(jnp.float32)).astype(x.dtype)


def head_rms_norm(y, w):
    y = y * lax.rsqrt(jnp.mean(y * y, axis=-1, keepdims=True) + RMS_EPS)
    return y.reshape(y.shape[:2] + (-1,)) * w.astype(jnp.float32)


def causal_dwconv(x, w, b):
    width, s = w.shape[0], x.shape[1]
    xp = jnp.pad(x, ((0, 0), (width - 1, 0), (0, 0)))
    out = b + xp[:, width - 1:width - 1 + s] * w[width - 1]
    for j in range(width - 1):
        out = out + xp[:, j:j + s] * w[j]
    return out


def soft_cap(z):
    return GATE_SOFT_CAP * jnp.tanh(z / GATE_SOFT_CAP)


def to_chunks(t, size):
    b, s = t.shape[:2]
    t = t.reshape((b, s // size, size) + t.shape[2:])
    return jnp.moveaxis(jnp.swapaxes(t, 2, 3), 1, 0)


def from_chunks(t):
    nc, b, h, l, d = t.shape
    return jnp.swapaxes(jnp.moveaxis(t, 0, 1), 2, 3).reshape(b, nc * l, h, d)


def mlstm_chunkwise(q, k, v, i_pre, f_pre):
    f32 = jnp.float32
    b, s = q.shape[:2]
    shp = (b, s, MLSTM_HEADS, MLSTM_HEAD_DIM)
    q = q.astype(f32).reshape(shp)
    k = k.astype(f32).reshape(shp) * (MLSTM_HEAD_DIM ** -0.5)
    v = v.astype(f32).reshape(shp)
    log_i = soft_cap(i_pre.astype(f32))
    log_f = jax.nn.log_sigmoid(soft_cap(f_pre.astype(f32)))
    causal = jnp.tril(jnp.ones((CHUNK, CHUNK), dtype=bool))

    def step(carry, xs):
        c_st, n_st, m_st = carry
        qc, kc, vc, ic, fc = xs
        bcum = jnp.cumsum(fc, axis=-1)
        dmat = jnp.where(causal, bcum[..., :, None] - bcum[..., None, :] + ic[..., None, :], NEG_BIG)
        m_inter = bcum + m_st[..., None]
        m_t = jnp.maximum(m_inter, jnp.max(dmat, axis=-1))
        w_intra = jnp.einsum('bhtd,bhsd->bhts', qc, kc) * jnp.exp(dmat - m_t[..., None])
        w_inter = jnp.exp(m_inter - m_t)
        num = (jnp.einsum('bhts,bhsv->bhtv', w_intra, vc)
               + w_inter[..., None] * jnp.einsum('bhtd,bhdv->bhtv', qc, c_st))
        den = jnp.sum(w_intra, axis=-1) + w_inter * jnp.einsum('bhtd,bhd->bht', qc, n_st)
        h = num / jnp.maximum(jnp.abs(den), jnp.exp(-m_t))[..., None]
        b_last = bcum[..., -1]
        g = b_last[..., None] - bcum + ic
        m_new = jnp.maximum(b_last + m_st, jnp.max(g, axis=-1))
        wk = jnp.exp(g - m_new[..., None])
        decay = jnp.exp(b_last + m_st - m_new)
        c_st = decay[..., None, None] * c_st + jnp.einsum('bhs,bhsd,bhsv->bhdv', wk, kc, vc)
        n_st = decay[..., None] * n_st + jnp.einsum('bhs,bhsd->bhd', wk, kc)
        return (c_st, n_st, m_new), h

    init = (jnp.zeros((b, MLSTM_HEADS, MLSTM_HEAD_DIM, MLSTM_HEAD_DIM), f32),
            jnp.zeros((b, MLSTM_HEADS, MLSTM_HEAD_DIM), f32),
            jnp.zeros((b, MLSTM_HEADS), f32))
    xs = (to_chunks(q, CHUNK), to_chunks(k, CHUNK), to_chunks(v, CHUNK),
          to_chunks(log_i, CHUNK), to_chunks(log_f, CHUNK))
    _, h = lax.scan(step, init, xs)
    return from_chunks(h)


def hgrn2_chunkwise(q_pre, f_pre, i_in, lower_bound):
    f32 = jnp.float32
    b, s = q_pre.shape[:2]
    kshp = (b, s, HGRN_HEADS, HGRN_EXPAND)
    q = jax.nn.silu(q_pre.astype(f32)).reshape(kshp)
    fp = f_pre.astype(f32).reshape(kshp)
    lb = lower_bound.astype(f32).reshape(HGRN_HEADS, HGRN_EXPAND)
    sig = jax.nn.sigmoid(fp)
    log_f = jnp.log(lb + (1.0 - lb) * sig)
    k = (1.0 - lb) * (1.0 - sig)
    v = i_in.astype(f32).reshape(b, s, HGRN_HEADS, HGRN_HEAD_DIM)
    causal = jnp.tril(jnp.ones((HGRN_BLOCK, HGRN_BLOCK), dtype=bool))[:, :, None]

    def step(s_st, xs):
        qc, kc, fc, vc = xs
        acum = jnp.cumsum(fc, axis=2)
        pair = jnp.exp(jnp.where(causal, acum[:, :, :, None, :] - acum[:, :, None, :, :], NEG_BIG))
        scores = jnp.einsum('bhtk,bhsk,bhtsk->bhts', qc, kc, pair)
        o = (jnp.einsum('bhts,bhsv->bhtv', scores, vc)
             + jnp.einsum('bhtk,bhkv->bhtv', qc * jnp.exp(acum), s_st))
        a_last = acum[:, :, -1]
        s_st = (jnp.exp(a_last)[..., None] * s_st
                + jnp.einsum('bhsk,bhsv->bhkv', kc * jnp.exp(a_last[:, :, None] - acum), vc))
        return s_st, o

    init = jnp.zeros((b, HGRN_HEADS, HGRN_EXPAND, HGRN_HEAD_DIM), f32)
    xs = (to_chunks(q, HGRN_BLOCK), to_chunks(k, HGRN_BLOCK),
          to_chunks(log_f, HGRN_BLOCK), to_chunks(v, HGRN_BLOCK))
    _, o = lax.scan(step, init, xs)
    return from_chunks(o)


def token_mixer(h, lower_bound, in_w, in_b, m_conv_w, m_conv_b, m_norm_w, h_norm_w, out_w):
    f32 = jnp.float32
    p = h @ in_w + in_b
    split_points = [int(t) for t in np.cumsum(PROJ_COLS)[:-1]]
    m_qk, m_v, m_o, m_i, m_f, h_q, h_f, h_i, h_g, g_a, g_b = jnp.split(p, split_points, axis=-1)
    m_qk = jax.nn.silu(causal_dwconv(m_qk, m_conv_w, m_conv_b))
    m_q, m_k = jnp.split(m_qk, 2, axis=-1)
    y_a = head_rms_norm(mlstm_chunkwise(m_q, m_k, m_v, m_i, m_f), m_norm_w) * jax.nn.sigmoid(m_o.astype(f32))
    y_b = head_rms_norm(hgrn2_chunkwise(h_q, h_f, h_i, lower_bound), h_norm_w) * jax.nn.silu(h_g.astype(f32))
    y = jax.nn.sigmoid(g_a.astype(f32)) * y_a + jax.nn.sigmoid(g_b.astype(f32)) * y_b
    return y.astype(h.dtype) @ out_w


def conv_ffn(h, up_w, conv_w, conv_b, down_w):
    u = causal_dwconv(h @ up_w, conv_w, conv_b)
    gate, val = jnp.split(u, 2, axis=-1)
    return (jax.nn.silu(gate) * val) @ down_w


def setup_inputs(seed: int = 0) -> dict:
    key = jax.random.key(seed)
    ks = jax.random.split(key, 19)

    def nrm(k, shape, scale):
        return jax.random.normal(k, shape, jnp.float32) * scale

    def gain(k, shape):
        return 1.0 + nrm(k, shape, 0.02)

    in_b = nrm(ks[6], (DEPTH, PROJ_WIDTH), 0.02)
    in_b = in_b.at[:, OFF_MLSTM_F:OFF_MLSTM_F + MLSTM_HEADS].add(
        jnp.linspace(3.0, 6.0, MLSTM_HEADS, dtype=jnp.float32))
    return {
        'x': nrm(ks[0], (BATCH, SEQ, D_MODEL), 1.0),
        'c': nrm(ks[1], (BATCH, D_MODEL), 1.0),
        'ada_w': nrm(ks[2], (DEPTH, D_MODEL, 6 * D_MODEL), 0.5 * D_MODEL ** -0.5),
        'ada_b': nrm(ks[3], (DEPTH, 6 * D_MODEL), 0.02),
        'mix_norm_w': gain(ks[4], (DEPTH, D_MODEL)),
        'in_w': nrm(ks[5], (DEPTH, D_MODEL, PROJ_WIDTH), D_MODEL ** -0.5),
        'in_b': in_b,
        'mlstm_conv_w': nrm(ks[7], (DEPTH, MLSTM_CONV, 2 * MLSTM_WIDTH), MLSTM_CONV ** -0.5),
        'mlstm_conv_b': nrm(ks[8], (DEPTH, 2 * MLSTM_WIDTH), 0.02),
        'mlstm_norm_w': gain(ks[9], (DEPTH, MLSTM_WIDTH)),
        'hgrn_lower_bounds': nrm(ks[10], (DEPTH, HGRN_KEY_WIDTH), 0.5),
        'hgrn_norm_w': gain(ks[11], (DEPTH, HGRN_WIDTH)),
        'out_w': nrm(ks[12], (DEPTH, D_MODEL, D_MODEL), D_MODEL ** -0.5),
        'ffn_norm_w': gain(ks[13], (DEPTH, D_MODEL)),
        'ffn_up_w': nrm(ks[14], (DEPTH, D_MODEL, 2 * D_FF), D_MODEL ** -0.5),
        'ffn_conv_w': nrm(ks[15], (DEPTH, FFN_CONV, 2 * D_FF), FFN_CONV ** -0.5),
        'ffn_conv_b': nrm(ks[16], (DEPTH, 2 * D_FF), 0.02),
        'ffn_down_w': nrm(ks[17], (DEPTH, D_FF, D_MODEL), D_FF ** -0.5),
        'final_norm_w': gain(ks[18], (D_MODEL,)),
    }


def reference(x, c, ada_w, ada_b, mix_norm_w, in_w, in_b, mlstm_conv_w, mlstm_conv_b,
              mlstm_norm_w, hgrn_lower_bounds, hgrn_norm_w, out_w, ffn_norm_w, ffn_up_w,
              ffn_conv_w, ffn_conv_b, ffn_down_w, final_norm_w):
    lb_soft = jax.nn.softmax(hgrn_lower_bounds.astype(jnp.float32), axis=0)
    lower_bounds = jnp.cumsum(lb_soft, axis=0) - lb_soft[0]
    c_act = jax.nn.silu(c)
    for layer in range(DEPTH):
        mod = (c_act @ ada_w[layer] + ada_b[layer])[:, None, :]
        shift_m, scale_m, gate_m, shift_f, scale_f, gate_f = jnp.split(mod, 6, axis=-1)
        h = rms_norm(x, mix_norm_w[layer]) * (1.0 + scale_m) + shift_m
        x = x + gate_m * token_mixer(h, lower_bounds[layer], in_w[layer], in_b[layer],
                                     mlstm_conv_w[layer], mlstm_conv_b[layer],
                                     mlstm_norm_w[layer], hgrn_norm_w[layer], out_w[layer])
        h = rms_norm(x, ffn_norm_w[layer]) * (1.0 + scale_f) + shift_f
        x = x + gate_f * conv_ffn(h, ffn_up_w[layer], ffn_conv_w[layer], ffn_conv_b[layer],
                                  ffn_down_w[layer])
    return rms_norm(x, final_norm_w)
```

```python
import numpy as np
import concourse.bass as bass
import concourse.mybir as mybir
from concourse.bass_utils import run_bass_kernel_spmd

F32 = mybir.dt.float32
BF16 = mybir.dt.bfloat16
AF = mybir.ActivationFunctionType
ALU = mybir.AluOpType
AX = mybir.AxisListType


class T:
    __slots__ = ("name", "t", "lw", "rd", "psum")

    def __init__(self, name, t, psum=False):
        self.name = name
        self.t = t
        self.lw = None
        self.rd = []
        self.psum = psum


class Op:
    __slots__ = ("eng", "fn", "reads", "writes", "chan", "deps", "sig", "ndma", "idx", "need_sig")

    def __init__(self, eng, fn, reads, writes, chan=None):
        self.eng = eng
        self.fn = fn
        self.reads = reads
        self.writes = writes
        self.chan = chan
        self.deps = []
        self.sig = None
        self.ndma = 0
        self.need_sig = False


class Prog:
    def __init__(self, nc):
        self.nc = nc
        self.ops = []
        self.chan_last = {}

    capture = None

    def op(self, eng, fn, reads=(), writes=()):
        o = Op(eng, fn, list(reads), list(writes))
        if self.capture is not None:
            self.capture.append(o)
        else:
            self._add(o)
        return o

    def replay(self, lst):
        for o in lst:
            self._add(o)

    def pipeline(self, segs):
        import os
        if os.environ.get("NOPIPE"):
            for sg in segs:
                self.replay(sg["A"]); self.replay(sg["B"]); self.replay(sg["C"])
            return
        if segs:
            self.replay(segs[0]["A"])
        for i, sg in enumerate(segs):
            if i + 1 < len(segs):
                self.replay(segs[i + 1]["A"])
            self.replay(sg["B"])
            self.replay(sg["C"])

    def dma(self, eng, chan, fn, reads=(), writes=()):
        o = Op(eng, fn, list(reads), list(writes), chan=chan)
        self._add(o)
        return o

    def _add(self, o):
        deps = []
        for t in o.reads:
            if t.lw is not None:
                deps.append(t.lw)
            if t.psum:
                deps.extend(r for r in t.rd if r.eng != o.eng)
        for t in o.writes:
            if t.lw is not None:
                deps.append(t.lw)
            deps.extend(t.rd)
        if o.chan is not None:
            p = self.chan_last.get(o.chan)
            if p is not None:
                deps.append(p)
            self.chan_last[o.chan] = o
        for t in o.reads:
            t.rd.append(o)
        for t in o.writes:
            t.lw = o
            t.rd = []
        seen = set()
        for d in deps:
            if id(d) in seen or d is o:
                continue
            seen.add(id(d))
            if d.chan is None and o.chan is None and d.eng == "pe" and o.eng == "pe":
                continue
            o.deps.append(d)
            d.need_sig = True
        o.idx = len(self.ops)
        self.ops.append(o)

    def finish(self):
        nc = self.nc
        engs = {"pe": nc.tensor, "act": nc.scalar, "dve": nc.vector, "pool": nc.gpsimd, "sp": nc.sync}
        fin = Op("sp", None, [], [])
        fin.deps = [o for o in self.chan_last.values()]
        fin.idx = len(self.ops)
        self.ops.append(fin)
        esem = {k: nc.alloc_semaphore("s_" + k) for k in engs}
        csem = {}
        ccount = {}
        ecount = {k: 0 for k in engs}
        for o in self.ops:
            if o.chan is not None:
                if o.chan not in csem:
                    csem[o.chan] = nc.alloc_semaphore("c_" + o.chan)
                    ccount[o.chan] = 0
                n = getattr(o.fn, "ndma", 1)
                ccount[o.chan] += 16 * n
                o.sig = (csem[o.chan], ccount[o.chan])
            elif o.need_sig:
                ecount[o.eng] += 1
                o.sig = (esem[o.eng], ecount[o.eng])
        self.nsem = len(esem) + len(csem)
        by_eng = {k: [] for k in engs}
        for o in self.ops:
            by_eng[o.eng].append(o)

        def emit(name, e):
            waited = {}
            for o in by_eng[name]:
                for d in o.deps:
                    sem, val = d.sig
                    key = id(sem)
                    if waited.get(key, 0) >= val:
                        continue
                    waited[key] = val
                    e.wait_ge(sem, val)
                if o.fn is None:
                    continue
                r = o.fn(e)
                if o.chan is not None:
                    assert len(r) == getattr(o.fn, "ndma", 1), (len(r), o.chan)
                    for ins in r:
                        ins.then_inc(o.sig[0], 16)
                elif o.sig is not None:
                    r.then_inc(o.sig[0], 1)

        with nc.Block() as block:
            @block.tensor
            def _(e):
                emit("pe", e)

            @block.scalar
            def _(e):
                emit("act", e)

            @block.vector
            def _(e):
                emit("dve", e)

            @block.gpsimd
            def _(e):
                emit("pool", e)

            @block.sync
            def _(e):
                emit("sp", e)
        self.counts = ecount


D = 1024
KC = 8
TT = 512
HEADS = 8
PW = 10256
FF = 2816
FFC = 22
DEPTH = 2
NSLAB = 42
SLABW = 9 * 512
EPS = 1e-6
CAP = 15.0
LNSCALE = float(np.log(128.0 ** -0.5))
MASKNEG = -10000.0
C_QK, C_V, C_O, C_I, C_F, C_HQ, C_HF, C_HI, C_HG, C_GA, C_GB = (
    0, 2048, 3072, 4096, 4104, 4112, 5136, 6160, 7184, 8208, 9232)


def vec_layout():
    off = {}
    n = 0

    def add(name, w):
        nonlocal n
        off[name] = n
        n += w
    for l in range(DEPTH):
        add(f"mixw{l}", 8); add(f"ffnw{l}", 8); add(f"inbqk{l}", 16); add(f"inbhq{l}", 8)
        add(f"inbhf{l}", 8); add(f"cw{l}", 64); add(f"cb{l}", 16); add(f"lbraw{l}", 8)
        add(f"fcw{l}", 132); add(f"fcb{l}", 44); add(f"adab{l}", 48); add(f"inbi{l}", 1); add(f"inbf{l}", 1)
    add("finw", 8)
    return off, n


def _fm(v):
    v = np.asarray(v, np.float32)
    return np.ascontiguousarray(v.reshape(-1, 128).T)


def _slab8(w):
    return np.ascontiguousarray(w.reshape(8, 128, 512).transpose(1, 0, 2)).reshape(128, 4096)


def prep_shared(inp):
    off, nv = vec_layout()
    V = np.zeros((128, nv), np.float32)
    wsl = np.zeros((DEPTH, NSLAB, 128, SLABW), np.float32)
    adaw = np.zeros((DEPTH, 48, 128, 8 * 128), np.float32)
    nrm = np.zeros((DEPTH, 2, D), np.float32)
    for l in range(DEPTH):
        inb = inp["in_b"][l]
        V[:, off[f"mixw{l}"]:off[f"mixw{l}"] + 8] = _fm(inp["mix_norm_w"][l])
        V[:, off[f"ffnw{l}"]:off[f"ffnw{l}"] + 8] = _fm(inp["ffn_norm_w"][l])
        V[:, off[f"inbqk{l}"]:off[f"inbqk{l}"] + 16] = _fm(inb[C_QK:C_QK + 2048])
        V[:, off[f"inbhq{l}"]:off[f"inbhq{l}"] + 8] = _fm(inb[C_HQ:C_HQ + 1024])
        V[:, off[f"inbhf{l}"]:off[f"inbhf{l}"] + 8] = _fm(inb[C_HF:C_HF + 1024])
        for j in range(4):
            V[:, off[f"cw{l}"] + 16 * j:off[f"cw{l}"] + 16 * j + 16] = _fm(inp["mlstm_conv_w"][l, j])
        V[:, off[f"cb{l}"]:off[f"cb{l}"] + 16] = _fm(inp["mlstm_conv_b"][l])
        V[:, off[f"lbraw{l}"]:off[f"lbraw{l}"] + 8] = _fm(inp["hgrn_lower_bounds"][l])
        for j in range(3):
            V[:, off[f"fcw{l}"] + 44 * j:off[f"fcw{l}"] + 44 * j + 44] = _fm(inp["ffn_conv_w"][l, j])
        V[:, off[f"fcb{l}"]:off[f"fcb{l}"] + 44] = _fm(inp["ffn_conv_b"][l])
        V[:, off[f"adab{l}"]:off[f"adab{l}"] + 48] = _fm(inp["ada_b"][l])
        V[0:8, off[f"inbi{l}"]] = inb[C_I:C_I + 8]
        V[0:8, off[f"inbf{l}"]] = inb[C_F:C_F + 8]
        W = inp["in_w"][l]
        fm_cols = [C_QK, C_QK + 512, C_QK + 1024, C_QK + 1536, C_HQ, C_HQ + 512, C_HF, C_HF + 512]
        for s, c0 in enumerate(fm_cols):
            wsl[l, s, :, :4096] = _slab8(W[:, c0:c0 + 512])
        tm_cols = [C_V, C_V + 512, C_O, C_O + 512, C_GA, C_GA + 512, C_HI, C_HI + 512,
                   C_HG, C_HG + 512, C_GB, C_GB + 512]
        for i, c0 in enumerate(tm_cols):
            wsl[l, 8 + i, :, :4096] = _slab8(W[:, c0:c0 + 512])
            wsl[l, 8 + i, 0, 4096:4608] = inb[c0:c0 + 512]
        Wo = inp["out_w"][l]
        wsl[l, 20, :, :4096] = _slab8(Wo[:, 0:512])
        wsl[l, 21, :, :4096] = _slab8(Wo[:, 512:1024])
        Wu = inp["ffn_up_w"][l]
        for s in range(11):
            cols = np.concatenate([np.arange(256 * s, 256 * s + 256), FF + np.arange(256 * s, 256 * s + 256)])
            wsl[l, 22 + s, :, :4096] = _slab8(Wu[:, cols])
        Wd = inp["ffn_down_w"][l]
        for jo in range(8):
            blk = Wd[:, jo * 128:(jo + 1) * 128].reshape(FFC, 128, 128).transpose(1, 0, 2)
            wsl[l, 33 + jo, :, :FFC * 128] = blk.reshape(128, FFC * 128)
        wif = W[:, C_I:C_I + 16].reshape(8, 128, 16).transpose(1, 0, 2)
        wsl[l, 41, :, :128] = wif.reshape(128, 128)
        Wa = inp["ada_w"][l]
        for j in range(48):
            adaw[l, j] = Wa[:, j * 128:(j + 1) * 128].reshape(8, 128, 128).transpose(1, 0, 2).reshape(128, 1024)
        nrm[l, 0] = inp["mlstm_norm_w"][l]
        nrm[l, 1] = inp["hgrn_norm_w"][l]
    V[:, off["finw"]:off["finw"] + 8] = _fm(inp["final_norm_w"])
    return {"vecs": V, "wsl": wsl, "adaw": adaw, "nrm": nrm}


def prep_core(x, c):
    nseq, S, _ = x.shape
    xT = np.ascontiguousarray(x.reshape(nseq, S, 8, 128).transpose(0, 3, 2, 1))
    cT = np.ascontiguousarray(c.reshape(nseq, 8, 128).transpose(2, 1, 0))
    return {"xT": xT, "cT": cT}


def unprep_out(oT):
    nseq, _, _, S = oT.shape
    return np.ascontiguousarray(oT.transpose(0, 3, 2, 1)).reshape(nseq, S, D)


def build_nc(NSEQ, S, NSLOT=2, dbg=None):
    assert S % TT == 0
    NT = S // TT
    off, NV = vec_layout()
    nc = bass.Bass("TRN2", target_bir_lowering=False)
    P = Prog(nc)

    xin = nc.dram_tensor("xT", [NSEQ, 128, 8, S], F32, kind="ExternalInput")
    cin = nc.dram_tensor("cT", [128, 8, NSEQ], F32, kind="ExternalInput")
    vin = nc.dram_tensor("vecs", [128, NV], F32, kind="ExternalInput")
    wsl = nc.dram_tensor("wsl", [DEPTH, NSLAB, 128, SLABW], F32, kind="ExternalInput")
    adw = nc.dram_tensor("adaw", [DEPTH, 48, 128, 1024], F32, kind="ExternalInput")
    nrm = nc.dram_tensor("nrm", [DEPTH, 2, D], F32, kind="ExternalInput")
    out = nc.dram_tensor("oT", [NSEQ, 128, 8, S], F32, kind="ExternalOutput")
    scr = nc.dram_tensor("scr", [DEPTH, NSLAB, 128, SLABW], BF16, kind="Internal")
    scrT = [[T(f"scr{l}_{s}", None) for s in range(NSLAB)] for l in range(DEPTH)]

    tot = [0]

    def sb(name, n, dt=F32):
        t = nc.alloc_sbuf_tensor(name, [128, n], dt)
        tot[0] += n * (4 if dt == F32 else 2)
        return T(name, t)

    def ps(name):
        t = nc.alloc_psum_tensor(name, [128, 512], F32)
        tt = T(name, t, True)
        return (tt, tt)

    def r3(ap, a):
        return ap.rearrange("p (a b) -> p a b", a=a)

    def bcast_mid(tl, p0, np_, offs, n_outer, outer_step, n_inner):
        base = tl.t[p0:p0 + np_, 0:1]
        pstep = base.ap[0][0]
        return bass.AP(tl.t, base.offset + offs, [[pstep, np_], [outer_step, n_outer], [0, n_inner]])

    V = sb("V", NV)
    identF = sb("identF", 128)
    identB = sb("identB", 128, BF16)
    onesB = sb("onesB", 128, BF16)
    negonesF = sb("negonesF", 128)
    lhsD = sb("lhsD", 512)
    rhsD = [sb(f"rhsD{i}", 512) for i in range(2)]
    negbm8 = sb("negbm8", 512)
    mask01 = sb("mask01", 512, BF16)
    negh = sb("negh", 8)
    ones1 = sb("ones1", 1)
    modT = [sb(f"modT{l}", 48 * NSEQ) for l in range(DEPTH)]
    AT = sb("AT", DEPTH * 2 * 8)
    LB = sb("LB", DEPTH * 8)
    OML = sb("OML", DEPTH * 8)
    NOML = sb("NOML", DEPTH * 8)
    gb15 = sb("gb15", DEPTH * 2)
    cs = sb("cs", 8 * NSEQ)
    Cext = [sb(f"Cext{l}", 1024) for l in range(DEPTH)]
    CextB = [sb(f"CextB{l}", 1024, BF16) for l in range(DEPTH)]
    nF = [sb(f"nF{l}", 8) for l in range(DEPTH)]
    nB = [sb(f"nB{l}", 8, BF16) for l in range(DEPTH)]
    Tst = [sb(f"Tst{l}", 1024) for l in range(DEPTH)]
    TstB = [sb(f"TstB{l}", 1024, BF16) for l in range(DEPTH)]
    tailqk = [sb(f"tailqk{l}", 16 * 3) for l in range(DEPTH)]
    tailff = [sb(f"tailff{l}", 44 * 2) for l in range(DEPTH)]
    carry = [sb(f"carry{l}", 2) for l in range(DEPTH)]
    xT_t = nc.alloc_sbuf_tensor("xT_sb", [128, 8 * 512], F32)
    tot[0] += 16384
    xTc = [T(f"xT{k}", xT_t) for k in range(8)]
    hT = sb("hT", 4096, BF16)
    yT = sb("yT", 4096, BF16)
    slabs = [sb(f"slab{i}", SLABW, BF16) for i in range(NSLOT)]
    bufQ = sb("bufQ", 4096, BF16)
    bufK = sb("bufK", 4096, BF16)
    xp = [sb(f"xp{i}", 516) for i in range(2)]
    acc = [sb(f"acc{i}", 512) for i in range(2)]
    GA = sb("GA", 512); GF = sb("GF", 512); GF2 = sb("GF2", 512)
    Bcat = sb("Bcat", 516); ucat = sb("ucat", 516)
    ARG = sb("ARG", 512); GE = sb("GE", 512)
    GT = sb("GT", 4 * 72)
    wibd = [sb(f"wibd{i}", 512) for i in range(2)]
    dec = [sb(f"dec{i}", 8) for i in range(2)]
    vtok = sb("vtok", 4096, BF16)
    Gp = sb("Gp", 4096, BF16)
    yacc = sb("yacc", 4096, BF16)
    tg = [sb(f"tg{i}", 512, BF16) for i in range(2)]
    nw = sb("nw", 1024)
    DTt = nc.alloc_sbuf_tensor("DT", [128, 512], F32); tot[0] += 2048
    DT = (T("DT_lo", DTt), T("DT_hi", DTt))
    wTt = nc.alloc_sbuf_tensor("wT", [128, 512], BF16); tot[0] += 1024
    wT = (T("wT_lo", wTt), T("wT_hi", wTt))
    qsc = [sb(f"qsc{i}", 512, BF16) for i in range(2)]
    kht = [nc.alloc_sbuf_tensor(f"khat{i}", [128, 1024], BF16) for i in range(2)]
    tot[0] += 4096
    khat = [(T(f"khat{i}_lo", kht[i]), T(f"khat{i}_hi", kht[i])) for i in range(2)]
    sqht = nc.alloc_sbuf_tensor("sqh", [128, 1024], F32); tot[0] += 4096
    sqh = (T("sqh_lo", sqht), T("sqh_hi", sqht))
    E01t = nc.alloc_sbuf_tensor("E01", [128, 1024], F32); tot[0] += 4096
    E01 = (T("E01_lo", E01t), T("E01_hi", E01t))
    smt = nc.alloc_sbuf_tensor("sm", [128, 64], F32); tot[0] += 256
    sm = (T("sm_lo", smt), T("sm_hi", smt))
    tmpF = [sb(f"tmpF{i}", 512) for i in range(4)]
    kkb = sb("kkb", 512, BF16); epb = sb("epb", 512, BF16); enb = sb("enb", 512, BF16)
    FA = sb("FA", 136); FX = sb("FX", 136)
    Qpad = [sb(f"Qpad{i}", 512, BF16) for i in range(2)]
    rm = sb("rm", 2)
    cbq = sb("cbq", DEPTH * 16)
    print("SBUF bytes/partition:", tot[0], "remaining", nc.sbuf_bytes_remaining)

    psA = ps("psA"); psB = ps("psB"); psS = ps("psS"); psD = ps("psD")
    psWI = ps("psWI"); psH0 = ps("psH0"); psH1 = ps("psH1"); psX = ps("psX")
    psAB = [psA, psB]

    def HS(hb):
        return slice(64 * hb, 64 * hb + 64)

    def vcol(name, j=0, n=1, p=128):
        return V.t[0:p, off[name] + j:off[name] + j + n]

    P.dma("sp", "misc", lambda e: [e.dma_start(out=V.t[:], in_=vin[:, :])], writes=[V])
    P.dma("sp", "misc", lambda e: [e.dma_start(out=r3(cs.t[:], 8), in_=cin[:, :, :])], writes=[cs])
    pool = "pool"
    P.op(pool, lambda e: e.memset(identF.t[:], 1.0), writes=[identF])
    P.op(pool, lambda e: e.affine_select(out=identF.t[:], in_=identF.t[:], pattern=[[-1, 128]],
                                         compare_op=ALU.is_equal, fill=0.0, base=0, channel_multiplier=1),
         reads=[identF], writes=[identF])
    P.op(pool, lambda e: e.tensor_copy(out=identB.t[:], in_=identF.t[:]), reads=[identF], writes=[identB])
    P.op(pool, lambda e: e.memset(onesB.t[:], 1.0), writes=[onesB])
    P.op(pool, lambda e: e.memset(negonesF.t[:], -1.0), writes=[negonesF])
    P.op(pool, lambda e: e.memset(negh.t[:], -0.5), writes=[negh])
    P.op(pool, lambda e: e.memset(ones1.t[:], 1.0), writes=[ones1])
    P.op(pool, lambda e: e.memset(lhsD.t[:], 0.0), writes=[lhsD])
    P.op(pool, lambda e: e.memset(lhsD.t[0:8, :], 1.0), writes=[lhsD])
    for c in range(8):
        P.op(pool, lambda e, c=c: e.tensor_copy(out=lhsD.t[64:128, c * 64:(c + 1) * 64], in_=identF.t[64:128, 64:128]),
             reads=[identF], writes=[lhsD])
    P.op(pool, lambda e: e.memset(negbm8.t[:], -1.0), writes=[negbm8])
    P.op(pool, lambda e: e.affine_select(out=r3(negbm8.t[0:8, :], 8), in_=r3(negbm8.t[0:8, :], 8),
                                         pattern=[[-1, 8], [0, 64]], compare_op=ALU.is_equal, fill=0.0,
                                         base=0, channel_multiplier=1), reads=[negbm8], writes=[negbm8])
    mb = tmpF[0]
    P.op(pool, lambda e: e.memset(mb.t[:], LNSCALE), writes=[mb])
    P.op(pool, lambda e: e.affine_select(out=r3(mb.t[0:64, :], 8), in_=r3(mb.t[0:64, :], 8),
                                         pattern=[[0, 8], [1, 64]], compare_op=ALU.is_ge, fill=MASKNEG,
                                         base=0, channel_multiplier=-1), reads=[mb], writes=[mb])
    m1 = tmpF[1]
    P.op(pool, lambda e: e.memset(m1.t[:], 1.0), writes=[m1])
    P.op(pool, lambda e: e.affine_select(out=r3(m1.t[0:64, :], 8), in_=r3(m1.t[0:64, :], 8),
                                         pattern=[[0, 8], [1, 64]], compare_op=ALU.is_ge, fill=0.0,
                                         base=0, channel_multiplier=-1), reads=[m1], writes=[m1])
    P.op(pool, lambda e: e.memset(r3(m1.t[0:32, :], 8)[:, :, 32:64], 0.0), reads=[m1], writes=[m1])
    P.op(pool, lambda e: e.memset(rm.t[:, 0:1], 1.0), writes=[rm])
    P.op(pool, lambda e: e.memset(rm.t[:, 1:2], 0.0), writes=[rm])
    for b0 in (32, 96):
        P.op(pool, lambda e, b0=b0: e.memset(rm.t[b0:b0 + 32, 0:1], 0.0), writes=[rm])
        P.op(pool, lambda e, b0=b0: e.memset(rm.t[b0:b0 + 32, 1:2], 1.0), writes=[rm])
    for i in range(2):
        P.op(pool, lambda e, i=i: e.memset(Qpad[i].t[:, :], 0.0), writes=[Qpad[i]])
    P.op(pool, lambda e: e.tensor_copy(out=mask01.t[0:64, :], in_=m1.t[0:64, :]), reads=[m1], writes=[mask01])
    P.op(pool, lambda e: e.tensor_copy(out=mask01.t[64:128, :], in_=m1.t[0:64, :]), reads=[m1], writes=[mask01])
    for i in range(2):
        P.op(pool, lambda e, i=i: e.memset(rhsD[i].t[:], 0.0), writes=[rhsD[i]])
        P.op(pool, lambda e, i=i: e.tensor_scalar(out=rhsD[i].t[32:40, :], in0=negbm8.t[0:8, :], scalar1=-1.0,
                                                  scalar2=None, op0=ALU.mult), reads=[negbm8], writes=[rhsD[i]])
        P.op(pool, lambda e, i=i: e.tensor_copy(out=rhsD[i].t[64:128, :], in_=mb.t[0:64, :]), reads=[mb], writes=[rhsD[i]])
    P.op(pool, lambda e: e.memset(ARG.t[:], 0.0), writes=[ARG])
    P.op("dve", lambda e: e.tensor_tensor(out=LB.t[:, 8:16], in0=vcol("lbraw1", 0, 8), in1=vcol("lbraw0", 0, 8),
                                          op=ALU.subtract), reads=[V], writes=[LB])
    P.op("act", lambda e: e.activation(out=LB.t[:, 8:16], in_=LB.t[:, 8:16], func=AF.Sigmoid), reads=[LB], writes=[LB])
    P.op("dve", lambda e: e.memset(LB.t[:, 0:8], 0.0), reads=[LB], writes=[LB])
    P.op("dve", lambda e: e.tensor_scalar(out=OML.t[:], in0=LB.t[:], scalar1=-1.0, scalar2=1.0, op0=ALU.mult, op1=ALU.add),
         reads=[LB], writes=[OML])
    P.op("dve", lambda e: e.tensor_scalar(out=NOML.t[:], in0=OML.t[:], scalar1=-1.0, scalar2=None, op0=ALU.mult),
         reads=[OML], writes=[NOML])
    for l in range(DEPTH):
        P.op("dve", lambda e, l=l: e.tensor_scalar(out=gb15.t[0:8, 2 * l:2 * l + 1], in0=vcol(f"inbi{l}", 0, 1, 8),
                                                   scalar1=1.0 / CAP, scalar2=None, op0=ALU.mult), reads=[V], writes=[gb15])
        P.op("dve", lambda e, l=l: e.tensor_scalar(out=gb15.t[0:8, 2 * l + 1:2 * l + 2], in0=vcol(f"inbf{l}", 0, 1, 8),
                                                   scalar1=1.0 / CAP, scalar2=None, op0=ALU.mult), reads=[V], writes=[gb15])
    P.op("act", lambda e: e.activation(out=cs.t[:], in_=cs.t[:], func=AF.Silu), reads=[cs], writes=[cs])
    for l in range(DEPTH):
        P.op("dve", lambda e, l=l: e.tensor_tensor(out=cbq.t[:, l * 16:(l + 1) * 16], in0=vcol(f"inbqk{l}", 0, 16),
                                                   in1=V.t[:, off[f"cw{l}"] + 48:off[f"cw{l}"] + 64], op=ALU.mult), reads=[V], writes=[cbq])
        P.op("dve", lambda e, l=l: e.tensor_tensor(out=cbq.t[:, l * 16:(l + 1) * 16], in0=cbq.t[:, l * 16:(l + 1) * 16],
                                                   in1=vcol(f"cb{l}", 0, 16), op=ALU.add), reads=[V, cbq], writes=[cbq])

    for l in range(DEPTH):
        for s in range(NSLAB):
            P.dma("pool", f"cast{(l * NSLAB + s) % 6}",
                  lambda e, l=l, s=s: [e.dma_start(out=scr[l, s, :, :], in_=wsl[l, s, :, :])], writes=[scrT[l][s]])

    AW = [T("AW0", sqht), T("AW1", E01t)]
    for l in range(DEPTH):
        for j in range(48):
            a = AW[j % 2]
            P.dma("sp", f"aw{j % 2}", lambda e, l=l, j=j, a=a: [e.dma_start(out=a.t[:, :], in_=adw[l, j, :, :])],
                  writes=[a, sqh[0], sqh[1]] if j % 2 == 0 else [a, E01[0], E01[1]])

            def mm(e, l=l, j=j, a=a):
                r = None
                for k in range(8):
                    r = e.matmul(psA[0].t[:, j * NSEQ:(j + 1) * NSEQ], lhsT=a.t[:, k * 128:(k + 1) * 128],
                                 rhs=r3(cs.t[:], 8)[:, k, :], start=(j == 0 and k == 0), stop=(k == 7),
                                 skip_group_check=True)
                return r
            P.op("pe", mm, reads=[a, cs], writes=[psA[0], psA[1]])
        P.op("dve", lambda e, l=l: e.tensor_tensor(
            out=r3(modT[l].t[:], 48), in0=r3(psA[0].t[:, 0:48 * NSEQ], 48),
            in1=bcast_mid(V, 0, 128, off[f"adab{l}"], 48, 1, NSEQ), op=ALU.add),
            reads=[psA[0], psA[1], V], writes=[modT[l]])

    order = [0, 1, 2, 3, 41, 8, 9, 10, 11, 12, 13, 4, 5, 6, 7, 14, 15, 16, 17, 18, 19, 20, 21] + \
        list(range(22, 33)) + list(range(33, 41))
    width = {s: 4096 for s in range(NSLAB)}
    for s in range(8, 20):
        width[s] = 4608
    for s in range(33, 41):
        width[s] = FFC * 128
    width[41] = 128
    reqs = []
    for _sq in range(NSEQ):
        for _ti in range(NT):
            for l in range(DEPTH):
                for s in order:
                    reqs.append((l, s))
    rq = {"issued": 0, "next": 0}

    def issue_until(n):
        while rq["issued"] < min(n, len(reqs)):
            i = rq["issued"]
            l, s = reqs[i]
            sl = slabs[i % NSLOT]
            w = width[s]
            P.dma("sp", f"slab{i % NSLOT}",
                  lambda e, l=l, s=s, sl=sl, w=w: [e.dma_start(out=sl.t[:, 0:w], in_=scr[l, s, :, 0:w])],
                  reads=[scrT[l][s]], writes=[sl])
            rq["issued"] += 1

    def next_slab(l, s):
        i = rq["next"]
        assert reqs[i] == (l, s), (reqs[i], l, s)
        issue_until(i + NSLOT)
        rq["next"] += 1
        return slabs[i % NSLOT]

    cnt = {"ps": 0, "xp": 0, "tg": 0, "c": 0}

    psROT = [psA, psB, psS, psD, psH0, psH1]

    def nextps():
        cnt["ps"] += 1
        return psROT[cnt["ps"] % len(psROT)]

    rs, rstd = tmpF[0], tmpF[1]
    tmpk = [tmpF[2], tmpF[3]]

    def norm(dst_fn, dst_T, scale_fn, bias_fn, extra_reads):
        xall = r3(xT_t[:, :], 8)
        P.op("act", lambda e: e.activation(out=yT.t[:], in_=xT_t[:, :], func=AF.Square), reads=xTc, writes=[yT])

        def mm(e):
            r = None
            for k in range(8):
                r = e.matmul(psA[0].t[:, :], lhsT=onesB.t[:, :], rhs=yT.t[:, k * 512:(k + 1) * 512],
                             start=(k == 0), stop=(k == 7))
            return r
        P.op("pe", mm, reads=[onesB, yT], writes=[psA[0], psA[1]])
        P.op("dve", lambda e: e.tensor_scalar(out=rs.t[:], in0=psA[0].t[:, :], scalar1=1.0 / D, scalar2=EPS,
                                              op0=ALU.mult, op1=ALU.add), reads=[psA[0], psA[1]], writes=[rs])
        P.op("act", lambda e: e.activation(out=rstd.t[:], in_=rs.t[:], func=AF.Ln), reads=[rs], writes=[rstd])
        P.op("act", lambda e: e.activation(out=rstd.t[:], in_=rstd.t[:], func=AF.Exp, scale=-0.5), reads=[rstd], writes=[rstd])
        for k in range(8):
            tk = tmpk[k % 2]
            P.op("dve", lambda e, k=k, tk=tk: e.tensor_tensor(out=tk.t[:], in0=xall[:, k, :], in1=rstd.t[:], op=ALU.mult),
                 reads=[xTc[k], rstd], writes=[tk])
            P.op("act", lambda e, k=k, tk=tk: e.activation(out=dst_fn(k), in_=tk.t[:], func=AF.Identity,
                                                          scale=scale_fn(k), bias=bias_fn(k)),
                 reads=[tk] + extra_reads, writes=[dst_T(k)])

    def group_mm(pst, out_ap_fn, lhs_fn, rhs_fn, nk, reads, extra=None):
        def mm(e):
            r = None
            for k in range(nk):
                r = e.matmul(out_ap_fn(), lhsT=lhs_fn(k), rhs=rhs_fn(k), start=(k == 0),
                             stop=(k == nk - 1 and extra is None))
            if extra is not None:
                r = e.matmul(out_ap_fn(), lhsT=extra[0], rhs=extra[1], start=False, stop=True)
            return r
        P.op("pe", mm, reads=reads, writes=[pst[0], pst[1]])

    hT3 = r3(hT.t[:], 8)
    yT3 = r3(yT.t[:], 8)
    Q3 = r3(bufQ.t[:], 8)
    K3 = r3(bufK.t[:], 8)
    xall = r3(xT_t[:, :], 8)
    vt3 = r3(vtok.t[:], 4)
    Gp3 = r3(Gp.t[:], 4)
    ya3 = r3(yacc.t[:], 4)

    def tm_slab(l, s, evac):
        sl = next_slab(l, s)
        sl3 = sl.t[:, 0:4096].rearrange("p (k c) -> p k c", k=8)
        for j in range(4):
            pst = nextps()
            group_mm(pst, lambda pst=pst: pst[0].t[:, :], lambda k, j=j: hT3[:, k, j * 128:(j + 1) * 128],
                     lambda k, sl3=sl3: sl3[:, k, :], 8, [hT, sl],
                     extra=(onesB.t[0:1, 0:128], sl.t[0:1, 4096:4608]))
            evac(j, pst)

    def conv_chunk(pst, xpT, accT, ntap, tailT, tidx, wname, bname, cidx, nch, bias_ap, l):
        nt = ntap - 1
        P.op("pool", lambda e: e.tensor_copy(out=xpT.t[:, 0:nt], in_=tailT.t[:, tidx * nt:(tidx + 1) * nt]),
             reads=[tailT], writes=[xpT])
        if bias_ap is None:
            P.op("act", lambda e: e.activation(out=xpT.t[:, nt:nt + 512], in_=pst[0].t[:, :], func=AF.Identity),
                 reads=[pst[0], pst[1]], writes=[xpT])
        else:
            P.op("act", lambda e: e.activation(out=xpT.t[:, nt:nt + 512], in_=pst[0].t[:, :], func=AF.Identity, bias=bias_ap),
                 reads=[pst[0], pst[1], V], writes=[xpT])
        P.op("pool", lambda e: e.tensor_copy(out=tailT.t[:, tidx * nt:(tidx + 1) * nt], in_=xpT.t[:, 512:512 + nt]),
             reads=[xpT], writes=[tailT])
        wcol = lambda j: V.t[:, off[wname] + nch * j + cidx:off[wname] + nch * j + cidx + 1]
        bcol = V.t[:, off[bname] + cidx:off[bname] + cidx + 1]
        if bias_ap is not None:
            bcol = cbq.t[:, l * 16 + cidx:l * 16 + cidx + 1]
        P.op("act", lambda e: e.activation(out=accT.t[:], in_=pst[0].t[:, :], func=AF.Identity, scale=wcol(nt), bias=bcol),
             reads=[pst[0], pst[1], V, cbq], writes=[accT])
        for j in range(nt):
            P.op("dve", lambda e, j=j: e.scalar_tensor_tensor(out=accT.t[:], in0=xpT.t[:, j:j + 512], scalar=wcol(j),
                                                              in1=accT.t[:], op0=ALU.mult, op1=ALU.add),
                 reads=[xpT, V, accT], writes=[accT])

    def layer(l, sq):
        mcol = lambda j, k: modT[l].t[:, (j * 8 + k) * NSEQ + sq:(j * 8 + k) * NSEQ + sq + 1]

        def stop(label):
            if dbg and dbg.get("stop") == label and l == dbg.get("nl", DEPTH) - 1:
                pos = rq["next"] % len(order)
                if pos:
                    for s_ in order[pos:]:
                        next_slab(l, s_)
                return True
            return False
        norm(lambda k: hT3[:, k, :], lambda k: hT, lambda k: AT.t[:, (l * 2 + 0) * 8 + k:(l * 2 + 0) * 8 + k + 1],
             lambda k: mcol(0, k), [AT, modT[l]])
        for c in range(16):
            if c % 4 == 0:
                sl = next_slab(l, c // 4)
                sl3 = sl.t[:, 0:4096].rearrange("p (k c) -> p k c", k=8)
            pst = nextps()
            group_mm(pst, lambda pst=pst: pst[0].t[:, :], lambda k, sl3=sl3, c=c: sl3[:, k, (c % 4) * 128:(c % 4 + 1) * 128],
                     lambda k: hT3[:, k, :], 8, [hT, sl])
            cnt["xp"] += 1
            xpT, accT = xp[cnt["xp"] % 2], acc[cnt["xp"] % 2]
            conv_chunk(pst, xpT, accT, 4, tailqk[l], c, f"cw{l}", f"cb{l}", c, 16, vcol(f"inbqk{l}", c), l)
            dstT, dst3 = (bufQ, Q3) if c < 8 else (bufK, K3)
            P.op("act", lambda e, accT=accT, dst3=dst3, c=c: e.activation(out=dst3[:, c % 8, :], in_=accT.t[:], func=AF.Silu),
                 reads=[accT], writes=[dstT])
        sl = next_slab(l, 41)
        slg = sl.t[:, 0:128].rearrange("p (k c) -> p k c", k=8)
        group_mm(psA, lambda: psA[0].t[0:8, :], lambda k, slg=slg: slg[:, k, 0:8], lambda k: hT3[:, k, :], 8, [hT, sl])
        group_mm(psB, lambda: psB[0].t[0:8, :], lambda k, slg=slg: slg[:, k, 8:16], lambda k: hT3[:, k, :], 8, [hT, sl])
        P.op("act", lambda e: e.activation(out=GA.t[0:8, :], in_=psA[0].t[0:8, :], func=AF.Tanh, scale=1.0 / CAP,
                                           bias=gb15.t[0:8, 2 * l:2 * l + 1]), reads=[psA[0], gb15], writes=[GA])
        P.op("act", lambda e: e.activation(out=GF.t[0:8, :], in_=psB[0].t[0:8, :], func=AF.Tanh, scale=1.0 / CAP,
                                           bias=gb15.t[0:8, 2 * l + 1:2 * l + 2]), reads=[psB[0], gb15], writes=[GF])
        P.op("act", lambda e: e.activation(out=GF2.t[0:8, :], in_=GF.t[0:8, :], func=AF.Exp, scale=-CAP), reads=[GF], writes=[GF2])
        P.op("act", lambda e: e.activation(out=GF.t[0:8, :], in_=GF2.t[0:8, :], func=AF.Ln, bias=ones1.t[0:8, 0:1]),
             reads=[GF2, ones1], writes=[GF])
        P.op("dve", lambda e: e.tensor_copy(out=Bcat.t[0:8, 0:1], in_=carry[l].t[0:8, 0:1]), reads=[carry[l]], writes=[Bcat])
        P.op("dve", lambda e: e.tensor_copy(out=ucat.t[0:8, 0:1], in_=carry[l].t[0:8, 1:2]), reads=[carry[l]], writes=[ucat])
        P.op("dve", lambda e: e.tensor_tensor_scan(out=Bcat.t[0:8, 1:513], data0=ones1.t[0:8, 0:1].to_broadcast([8, 512]),
                                                   data1=GF.t[0:8, :], initial=Bcat.t[0:8, 0:1], op0=ALU.mult, op1=ALU.add),
             reads=[GF, Bcat, ones1], writes=[Bcat])
        P.op("dve", lambda e: e.scalar_tensor_tensor(out=GA.t[0:8, :], in0=GA.t[0:8, :], scalar=CAP, in1=Bcat.t[0:8, 1:513],
                                                     op0=ALU.mult, op1=ALU.add), reads=[GA, Bcat], writes=[GA])
        P.op("dve", lambda e: e.tensor_tensor_scan(out=ucat.t[0:8, 1:513], data0=GA.t[0:8, :], data1=GA.t[0:8, :],
                                                   initial=ucat.t[0:8, 0:1], op0=ALU.max, op1=ALU.max),
             reads=[GA, ucat], writes=[ucat])
        P.op("pool", lambda e: e.tensor_copy(out=lhsD.t[32:40, :], in_=GA.t[0:8, :]), reads=[GA], writes=[lhsD])
        P.op("dve", lambda e: e.tensor_tensor(out=r3(ARG.t[0:8, :], 8), in0=bcast_mid(ucat, 0, 8, 0, 8, 64, 64),
                                              in1=r3(ucat.t[0:8, 1:513], 8), op=ALU.subtract), reads=[ucat], writes=[ARG])
        P.op("dve", lambda e: e.tensor_tensor(out=ARG.t[32:40, :], in0=Bcat.t[0:8, 1:513], in1=ucat.t[0:8, 1:513],
                                              op=ALU.subtract), reads=[ucat, Bcat], writes=[ARG])
        P.op("dve", lambda e: e.scalar_tensor_tensor(out=r3(ARG.t[64:72, :], 8), in0=r3(GA.t[0:8, :], 8), scalar=LNSCALE,
                                                     in1=bcast_mid(ucat, 0, 8, 64, 8, 64, 64), op0=ALU.add, op1=ALU.subtract),
             reads=[GA, ucat], writes=[ARG])
        P.op("act", lambda e: e.activation(out=GE.t[0:72, :], in_=ARG.t[0:72, :], func=AF.Exp), reads=[ARG], writes=[GE])
        P.op("dve", lambda e: e.tensor_copy(out=carry[l].t[0:8, 0:1], in_=Bcat.t[0:8, 512:513]), reads=[Bcat], writes=[carry[l]])
        P.op("dve", lambda e: e.tensor_copy(out=carry[l].t[0:8, 1:2], in_=ucat.t[0:8, 512:513]), reads=[ucat], writes=[carry[l]])

        def gtr(e):
            r = None
            for j in range(4):
                r = e.transpose(psA[0].t[:, j * 72:(j + 1) * 72], GE.t[0:72, j * 128:(j + 1) * 128], identF.t[0:72, 0:72])
            return r
        P.op("pe", gtr, reads=[GE, identF], writes=[psA[0], psA[1]])
        P.op("dve", lambda e: e.tensor_copy(out=GT.t[:, :], in_=psA[0].t[:, 0:288]), reads=[psA[0], psA[1]], writes=[GT])
        GT3 = r3(GT.t[:], 4)
        P.dma("sp", "nw", lambda e: [e.dma_start(out=nw.t[:, :], in_=nrm[l, 0:1, :].partition_broadcast(128))], writes=[nw])
        for i in range(2):
            tm_slab(l, 8 + i, lambda j, pst, i=i: P.op(
                "act", lambda e: e.activation(out=vt3[:, j, i * 512:(i + 1) * 512], in_=pst[0].t[:, :], func=AF.Identity),
                reads=[pst[0], pst[1]], writes=[vtok]))
        for i in range(2):
            tm_slab(l, 10 + i, lambda j, pst, i=i: P.op(
                "act", lambda e: e.activation(out=Gp3[:, j, i * 512:(i + 1) * 512], in_=pst[0].t[:, :], func=AF.Sigmoid),
                reads=[pst[0], pst[1]], writes=[Gp]))

        def gate2(j, pst, i, func):
            cnt["tg"] += 1
            tgt = tg[cnt["tg"] % 2]
            P.op("act", lambda e: e.activation(out=tgt.t[:], in_=pst[0].t[:, :], func=func),
                 reads=[pst[0], pst[1]], writes=[tgt])
            P.op("dve", lambda e: e.tensor_tensor(out=Gp3[:, j, i * 512:(i + 1) * 512], in0=Gp3[:, j, i * 512:(i + 1) * 512],
                                                  in1=tgt.t[:], op=ALU.mult), reads=[Gp, tgt], writes=[Gp])
            P.op("dve", lambda e: e.tensor_tensor(out=Gp3[:, j, i * 512:(i + 1) * 512], in0=Gp3[:, j, i * 512:(i + 1) * 512],
                                                  in1=nw.t[:, i * 512:(i + 1) * 512], op=ALU.mult), reads=[Gp, nw], writes=[Gp])
        for i in range(2):
            tm_slab(l, 12 + i, lambda j, pst, i=i: gate2(j, pst, i, AF.Sigmoid))

        CB3 = r3(CextB[l].t[:], 8)
        C3 = r3(Cext[l].t[:], 8)
        msegs = []
        for c in range(8):
            hb, j = c % 2, c // 2
            hs = HS(hb)
            cnt["c"] += 1
            sl_ = cnt["c"] % 2
            rD, wb, qs_, dc, kh = rhsD[sl_], wibd[sl_], qsc[sl_], dec[sl_], khat[sl_]
            c0 = c * 64
            sg = {"A": [], "B": [], "C": []}
            msegs.append(sg)
            P.capture = sg["A"]
            P.op("dve", lambda e, c0=c0, rD=rD: e.tensor_tensor(
                out=r3(rD.t[0:8, :], 8), in0=bass.AP(ucat.t, ucat.t[0:8, 1 + c0:2 + c0].offset,
                                                     [[ucat.t[0:8, 0:1].ap[0][0], 8], [0, 8], [1, 64]]),
                in1=r3(negbm8.t[0:8, :], 8), op=ALU.mult), reads=[ucat, negbm8], writes=[rD])
            P.op("dve", lambda e, c0=c0, wb=wb: e.tensor_tensor(
                out=r3(wb.t[0:8, :], 8), in0=bass.AP(GE.t, GE.t[0:8, c0:c0 + 1].offset,
                                                     [[GE.t[0:8, 0:1].ap[0][0], 8], [0, 8], [1, 64]]),
                in1=r3(negbm8.t[0:8, :], 8), op=ALU.mult), reads=[GE, negbm8], writes=[wb])

            def smm(e, c0=c0, hs=hs, hb=hb):
                r = None
                for h in range(8):
                    r = e.matmul(psS[hb].t[hs, h * 64:(h + 1) * 64], lhsT=K3[:, h, c0:c0 + 64], rhs=Q3[:, h, c0:c0 + 64],
                                 start=True, stop=True, skip_group_check=True)
                return r
            P.op("pe", smm, reads=[bufQ, bufK], writes=[psS[hb]])
            P.op("pe", lambda e, c0=c0, hs=hs, hb=hb, rD=rD: e.matmul(psD[hb].t[hs, :], lhsT=lhsD.t[:, c0:c0 + 64], rhs=rD.t[:, :],
                                                               start=True, stop=True), reads=[lhsD, rD], writes=[psD[hb]])
            P.op("pe", lambda e, wb=wb: e.matmul(psWI[0].t[:, :], lhsT=negonesF.t[0:8, :], rhs=wb.t[0:8, :], start=True, stop=True),
                 reads=[negonesF, wb], writes=[psWI[0], psWI[1]])
            P.op("act", lambda e, hs=hs, hb=hb: e.activation(out=DT[hb].t[hs, :], in_=psD[hb].t[hs, :], func=AF.Exp),
                 reads=[psD[hb]], writes=[DT[hb]])
            P.op("dve", lambda e, hs=hs, hb=hb: e.tensor_tensor(out=wT[hb].t[hs, :], in0=psS[hb].t[hs, :], in1=DT[hb].t[hs, :],
                                                                op=ALU.mult), reads=[psS[hb], DT[hb]], writes=[wT[hb]])
            P.op("dve", lambda e, c0=c0, qs_=qs_: e.tensor_tensor(out=r3(qs_.t[:], 8), in0=Q3[:, :, c0:c0 + 64],
                                                                  in1=r3(psWI[0].t[:, :], 8), op=ALU.mult),
                 reads=[bufQ, psWI[0], psWI[1]], writes=[qs_])
            P.op("dve", lambda e, dc=dc: e.tensor_copy(out=dc.t[:, :], in_=r3(psWI[0].t[:, :], 8)[:, :, 63]),
                 reads=[psWI[0], psWI[1]], writes=[dc])
            qs3 = r3(qs_.t[:], 8)

            def hmm(e, hs=hs, hb=hb, j=j, qs3=qs3):
                r = None
                for h in range(8):
                    pH = (psH0, psH1)[h // 4][hb]
                    o_ = pH.t[hs, (h % 4) * 128:(h % 4 + 1) * 128]
                    e.matmul(o_, lhsT=wT[hb].t[hs, h * 64:(h + 1) * 64], rhs=vt3[hs, j, h * 128:(h + 1) * 128],
                             start=(h % 4 == 0), stop=False, skip_group_check=True)
                    r = e.matmul(o_, lhsT=qs3[:, h, :], rhs=CB3[:, h, :], start=False, stop=True, skip_group_check=True)
                for h in range(8):
                    o_ = psX[hb].t[hs, 256 + h:257 + h]
                    e.matmul(o_, lhsT=wT[hb].t[hs, h * 64:(h + 1) * 64], rhs=onesB.t[hs, 0:1],
                             start=(h == 0), stop=False, skip_group_check=True)
                    r = e.matmul(o_, lhsT=qs3[:, h, :], rhs=nB[l].t[:, h:h + 1], start=False, stop=True, skip_group_check=True)
                return r
            P.capture = sg["B"]
            P.op("pe", hmm, reads=[wT[hb], vtok, qs_, CextB[l], nB[l], onesB], writes=[psH0[hb], psH1[hb], psX[hb]])
            S_ = sm[hb].t
            P.op("act", lambda e, hs=hs, hb=hb: e.activation(out=E01t[hs, 0:512], in_=psH0[hb].t[hs, :], func=AF.Identity),
                 reads=[psH0[hb]], writes=[E01[hb]])
            P.op("act", lambda e, hs=hs, hb=hb: e.activation(out=E01t[hs, 512:1024], in_=psH1[hb].t[hs, :], func=AF.Identity),
                 reads=[psH1[hb]], writes=[E01[hb]])
            P.op("dve", lambda e, hs=hs, hb=hb, S_=S_: e.tensor_copy(out=S_[hs, 56:64], in_=psX[hb].t[hs, 256:264]),
                 reads=[psX[hb]], writes=[sm[hb]])
            P.capture = sg["C"]
            P.op("dve", lambda e, hs=hs, hb=hb, j=j, S_=S_: e.tensor_tensor(out=S_[hs, 48:56], in0=S_[hs, 56:64],
                                                                        in1=GT3[hs, j, 32:40], op=ALU.max),
                 reads=[sm[hb], GT], writes=[sm[hb]])
            P.op("dve", lambda e, hs=hs, hb=hb, S_=S_: e.scalar_tensor_tensor(out=S_[hs, 0:8], in0=S_[hs, 56:64], scalar=-1.0,
                                                                          in1=S_[hs, 48:56], op0=ALU.mult, op1=ALU.max),
                 reads=[sm[hb]], writes=[sm[hb]])
            P.op("dve", lambda e, hs=hs, S_=S_: e.reciprocal(out=S_[hs, 8:16], in_=S_[hs, 0:8]), reads=[sm[hb]], writes=[sm[hb]])
            P.op("act", lambda e, hs=hs, hb=hb: e.activation(out=sqht[hs, :], in_=E01t[hs, :], func=AF.Square),
                 reads=[E01[hb]], writes=[sqh[hb]])
            P.op("dve", lambda e, hs=hs, S_=S_: e.tensor_reduce(out=S_[hs, 16:24], in_=r3(sqht[hs, :], 8), axis=AX.X, op=ALU.add),
                 reads=[sqh[hb]], writes=[sm[hb]])
            P.op("dve", lambda e, hs=hs, S_=S_: e.tensor_tensor(out=S_[hs, 24:32], in0=S_[hs, 8:16], in1=S_[hs, 8:16], op=ALU.mult),
                 reads=[sm[hb]], writes=[sm[hb]])
            P.op("dve", lambda e, hs=hs, S_=S_: e.tensor_tensor(out=S_[hs, 24:32], in0=S_[hs, 24:32], in1=S_[hs, 16:24], op=ALU.mult),
                 reads=[sm[hb]], writes=[sm[hb]])
            P.op("dve", lambda e, hs=hs, S_=S_: e.tensor_scalar(out=S_[hs, 24:32], in0=S_[hs, 24:32], scalar1=1.0 / 128, scalar2=EPS,
                                                                op0=ALU.mult, op1=ALU.add), reads=[sm[hb]], writes=[sm[hb]])
            P.op("act", lambda e, hs=hs, S_=S_: e.activation(out=S_[hs, 32:40], in_=S_[hs, 24:32], func=AF.Ln),
                 reads=[sm[hb]], writes=[sm[hb]])
            P.op("act", lambda e, hs=hs, S_=S_: e.activation(out=S_[hs, 32:40], in_=S_[hs, 32:40], func=AF.Exp, scale=-0.5),
                 reads=[sm[hb]], writes=[sm[hb]])
            P.op("dve", lambda e, hs=hs, S_=S_: e.tensor_tensor(out=S_[hs, 40:48], in0=S_[hs, 32:40], in1=S_[hs, 8:16], op=ALU.mult),
                 reads=[sm[hb]], writes=[sm[hb]])
            P.op("dve", lambda e, hs=hs, S_=S_: e.tensor_tensor(
                out=r3(E01t[hs, :], 8), in0=r3(E01t[hs, :], 8),
                in1=bass.AP(smt, S_[hs, 40:41].offset, [[S_[hs, 0:1].ap[0][0], 64], [1, 8], [0, 128]]),
                op=ALU.mult), reads=[sm[hb]], writes=[E01[hb]])
            P.op("dve", lambda e, hs=hs, j=j: e.tensor_tensor(
                out=ya3[hs, j, :], in0=E01t[hs, :], in1=Gp3[hs, j, :], op=ALU.mult), reads=[E01[hb], Gp], writes=[yacc])
            P.capture = sg["A"]
            psXb = psX[hb].t[:, :].bitcast(BF16)
            kh3 = r3(kh[hb].t[hs, :], 8)
            for q in range(2):
                def ktr(e, q=q, c0=c0, hs=hs, psXb=psXb):
                    r = None
                    for hh in range(4):
                        h = 4 * q + hh
                        r = e.transpose(psXb[hs, hh * 128:(hh + 1) * 128], K3[:, h, c0:c0 + 64], identB.t[:, :])
                    return r
                P.op("pe", ktr, reads=[bufK, identB], writes=[psX[hb]])
                P.op("dve", lambda e, q=q, hs=hs, j=j, psXb=psXb, kh3=kh3: e.tensor_tensor(
                    out=kh3[:, 4 * q:4 * q + 4, :], in0=r3(psXb[hs, 0:512], 4),
                    in1=bass.AP(GT.t, GT3[hs, j, 64 + 4 * q:65 + 4 * q].offset, [[GT.t[hs, 0:1].ap[0][0], 64], [1, 4], [0, 128]]),
                    op=ALU.mult), reads=[psX[hb], GT], writes=[kh[hb]])

            def cmm(e, hs=hs, j=j, kh3=kh3):
                r = None
                for h in range(8):
                    pc = psAB[h // 4][0]
                    r = e.matmul(pc.t[:, (h % 4) * 128:(h % 4 + 1) * 128], lhsT=kh3[:, h, :], rhs=vt3[hs, j, h * 128:(h + 1) * 128],
                                 start=(h % 4 == 0), stop=True, skip_group_check=True)
                for h in range(8):
                    r = e.matmul(psWI[0].t[:, h:h + 1], lhsT=kh3[:, h, :], rhs=onesB.t[hs, 0:1],
                                 start=(h == 0), stop=True, skip_group_check=True)
                return r
            P.capture = sg["B"]
            P.op("pe", cmm, reads=[kh[hb], vtok, onesB], writes=[psA[0], psA[1], psB[0], psB[1], psWI[0], psWI[1]])
            for h in range(8):
                pc = psAB[h // 4]
                P.op("dve", lambda e, h=h, pc=pc, dc=dc: e.scalar_tensor_tensor(
                    out=CB3[:, h, :], in0=C3[:, h, :], scalar=dc.t[:, h:h + 1], in1=pc[0].t[:, (h % 4) * 128:(h % 4 + 1) * 128],
                    op0=ALU.mult, op1=ALU.add), reads=[Cext[l], dc, pc[0], pc[1]], writes=[CextB[l]])
            P.op("dve", lambda e, dc=dc: e.tensor_tensor(out=nF[l].t[:, :], in0=nF[l].t[:, :], in1=dc.t[:, :], op=ALU.mult),
                 reads=[nF[l], dc], writes=[nF[l]])
            P.op("dve", lambda e: e.tensor_tensor(out=nF[l].t[:, :], in0=nF[l].t[:, :], in1=psWI[0].t[:, 0:8], op=ALU.add),
                 reads=[nF[l], psWI[0], psWI[1]], writes=[nF[l]])
            P.op("dve", lambda e: e.tensor_copy(out=nB[l].t[:, :], in_=nF[l].t[:, :]), reads=[nF[l]], writes=[nB[l]])
            for h in range(8):
                pc = psAB[h // 4]
                P.op("dve", lambda e, h=h, pc=pc, dc=dc: e.scalar_tensor_tensor(
                    out=C3[:, h, :], in0=C3[:, h, :], scalar=dc.t[:, h:h + 1], in1=pc[0].t[:, (h % 4) * 128:(h % 4 + 1) * 128],
                    op0=ALU.mult, op1=ALU.add), reads=[Cext[l], dc, pc[0], pc[1]], writes=[Cext[l]])
            P.capture = None
        P.pipeline(msegs)

        if dbg and dbg.get("stage") == "mlstm":
            for s_ in order[order.index(13) + 1:]:
                next_slab(l, s_)
            return
        slq = {}
        for which in range(2):
            for h in range(8):
                key = (which, h // 4)
                if key not in slq:
                    slq[key] = next_slab(l, (4 if which == 0 else 6) + h // 4)
                sl = slq[key]
                sl3 = sl.t[:, 0:4096].rearrange("p (k c) -> p k c", k=8)
                pst = nextps()
                group_mm(pst, lambda pst=pst: pst[0].t[:, :], lambda k, sl3=sl3, h=h: sl3[:, k, (h % 4) * 128:(h % 4 + 1) * 128],
                         lambda k: hT3[:, k, :], 8, [hT, sl])
                if which == 0:
                    P.op("act", lambda e, pst=pst, h=h: e.activation(out=Q3[:, h, :], in_=pst[0].t[:, :], func=AF.Silu,
                                                                    bias=vcol(f"inbhq{l}", h)), reads=[pst[0], pst[1], V], writes=[bufQ])
                else:
                    sg_, lf_, G_ = tmpF[0], tmpF[1], tmpF[2]
                    P.op("act", lambda e, pst=pst, h=h: e.activation(out=sg_.t[:], in_=pst[0].t[:, :], func=AF.Sigmoid,
                                                                    bias=vcol(f"inbhf{l}", h)), reads=[pst[0], pst[1], V], writes=[sg_])
                    P.op("act", lambda e, h=h: e.activation(out=lf_.t[:], in_=sg_.t[:], func=AF.Ln,
                                                           scale=OML.t[:, l * 8 + h:l * 8 + h + 1], bias=LB.t[:, l * 8 + h:l * 8 + h + 1]),
                         reads=[sg_, OML, LB], writes=[lf_])
                    P.op("dve", lambda e, h=h: e.tensor_scalar(out=kkb.t[:], in0=sg_.t[:], scalar1=NOML.t[:, l * 8 + h:l * 8 + h + 1],
                                                              scalar2=OML.t[:, l * 8 + h:l * 8 + h + 1], op0=ALU.mult, op1=ALU.add),
                         reads=[sg_, NOML, OML], writes=[kkb])
                    P.op("dve", lambda e: e.tensor_tensor_scan(out=G_.t[:], data0=ones1.t[:, 0:1].to_broadcast([128, 512]),
                                                               data1=lf_.t[:], initial=0.0, op0=ALU.mult, op1=ALU.add),
                         reads=[lf_, ones1], writes=[G_])
                    P.op("dve", lambda e: e.tensor_tensor(out=r3(lf_.t[:], 16), in0=r3(G_.t[:], 16),
                                                          in1=bcast_mid(G_, 0, 128, 15, 16, 32, 32), op=ALU.subtract),
                         reads=[G_], writes=[lf_])
                    FA3 = FA.t[:, :].rearrange("p (h n) -> p h n", h=8)
                    gstep = G_.t[:, 0:1].ap[0][0]
                    P.op("dve", lambda e, h=h: e.tensor_tensor(
                        out=FA3[:, h, 0:15], in0=bass.AP(G_.t, 47, [[gstep, 128], [32, 15]]),
                        in1=bass.AP(G_.t, 15, [[gstep, 128], [32, 15]]), op=ALU.subtract), reads=[G_], writes=[FA])
                    P.op("dve", lambda e, h=h: e.tensor_tensor(out=FA3[:, h, 15:16], in0=G_.t[:, 511:512], in1=G_.t[:, 495:496],
                                                              op=ALU.subtract), reads=[G_], writes=[FA])
                    P.op("dve", lambda e, h=h: e.tensor_copy(out=FA3[:, h, 16:17], in_=G_.t[:, 15:16]), reads=[G_], writes=[FA])
                    P.op("act", lambda e: e.activation(out=epb.t[:], in_=lf_.t[:], func=AF.Exp), reads=[lf_], writes=[epb])
                    P.op("act", lambda e: e.activation(out=enb.t[:], in_=lf_.t[:], func=AF.Exp, scale=-1.0), reads=[lf_], writes=[enb])
                    P.op("dve", lambda e, h=h: e.tensor_tensor(out=Q3[:, h, :], in0=Q3[:, h, :], in1=epb.t[:], op=ALU.mult),
                         reads=[bufQ, epb], writes=[bufQ])
                    P.op("dve", lambda e, h=h: e.tensor_tensor(out=K3[:, h, :], in0=kkb.t[:], in1=enb.t[:], op=ALU.mult),
                         reads=[kkb, enb], writes=[bufK])
        if stop("hg_in"):
            return
        P.op("act", lambda e: e.activation(out=FX.t[:, :], in_=FA.t[:, :], func=AF.Exp), reads=[FA], writes=[FX])
        FX3 = FX.t[:, :].rearrange("p (h n) -> p h n", h=8)
        P.dma("sp", "nw", lambda e: [e.dma_start(out=nw.t[:, :], in_=nrm[l, 1:2, :].partition_broadcast(128))], writes=[nw])
        for i in range(2):
            tm_slab(l, 14 + i, lambda j, pst, i=i: P.op(
                "act", lambda e: e.activation(out=vt3[:, j, i * 512:(i + 1) * 512], in_=pst[0].t[:, :], func=AF.Identity),
                reads=[pst[0], pst[1]], writes=[vtok]))
        for i in range(2):
            tm_slab(l, 16 + i, lambda j, pst, i=i: P.op(
                "act", lambda e: e.activation(out=Gp3[:, j, i * 512:(i + 1) * 512], in_=pst[0].t[:, :], func=AF.Silu),
                reads=[pst[0], pst[1]], writes=[Gp]))
        for i in range(2):
            tm_slab(l, 18 + i, lambda j, pst, i=i: gate2(j, pst, i, AF.Sigmoid))
        T3 = r3(Tst[l].t[:], 8)
        TB3 = r3(TstB[l].t[:], 8)
        fstep = FX.t[:, 0:1].ap[0][0]
        P.op("dve", lambda e: e.tensor_tensor(out=T3, in0=T3, in1=bass.AP(FX.t, 16, [[fstep, 128], [17, 8], [0, 128]]), op=ALU.mult),
             reads=[Tst[l], FX], writes=[Tst[l]])
        P.op("act", lambda e: e.activation(out=TstB[l].t[:, :], in_=Tst[l].t[:, :], func=AF.Identity), reads=[Tst[l]], writes=[TstB[l]])
        if stop("hg_tm"):
            return
        hsegs = []
        for pr in range(8):
            hb, j = pr % 2, pr // 2
            hs = HS(hb)
            cnt["c"] += 1
            kh0 = khat[0]
            kh1 = khat[1]
            qp = Qpad[cnt["c"] % 2]
            c0 = pr * 64
            sg = {"A": [], "B": [], "C": []}
            hsegs.append(sg)
            P.capture = sg["A"]

            def smm2(e, c0=c0, hs=hs, hb=hb):
                r = None
                for h in range(8):
                    r = e.matmul(psS[hb].t[hs, h * 64:(h + 1) * 64], lhsT=K3[:, h, c0:c0 + 64], rhs=Q3[:, h, c0:c0 + 64],
                                 start=True, stop=True, skip_group_check=True)
                return r
            P.op("pe", smm2, reads=[bufQ, bufK], writes=[psS[hb]])
            P.op("dve", lambda e, hs=hs, hb=hb: e.tensor_tensor(out=wT[hb].t[hs, :], in0=psS[hb].t[hs, :], in1=mask01.t[hs, :],
                                                                op=ALU.mult), reads=[psS[hb], mask01], writes=[wT[hb]])
            P.op("pool", lambda e, qp=qp, c0=c0: e.tensor_copy(out=r3(qp.t[:], 8)[:, :, 32:64], in_=Q3[:, :, c0 + 32:c0 + 64]),
                 reads=[bufQ], writes=[qp])
            psXb = psX[hb].t[:, :].bitcast(BF16)
            k03 = r3(kh0[hb].t[hs, :], 8)
            k13 = r3(kh1[hb].t[hs, :], 8)
            for q in range(2):
                def ktr2(e, q=q, c0=c0, hs=hs, psXb=psXb):
                    r = None
                    for hh in range(4):
                        h = 4 * q + hh
                        r = e.transpose(psXb[hs, hh * 128:(hh + 1) * 128], K3[:, h, c0:c0 + 64], identB.t[:, :])
                    return r
                P.op("pe", ktr2, reads=[bufK, identB], writes=[psX[hb]])
                P.op("act", lambda e, q=q, hs=hs, psXb=psXb, k03=k03: e.activation(
                    out=k03[:, 4 * q:4 * q + 4, :], in_=r3(psXb[hs, 0:512], 4), func=AF.Identity, scale=rm.t[hs, 0:1]),
                    reads=[psX[hb], rm], writes=[kh0[hb]])
                P.op("act", lambda e, q=q, hs=hs, psXb=psXb, k13=k13: e.activation(
                    out=k13[:, 4 * q:4 * q + 4, :], in_=r3(psXb[hs, 0:512], 4), func=AF.Identity, scale=rm.t[hs, 1:2]),
                    reads=[psX[hb], rm], writes=[kh1[hb]])

            def omm(e, hs=hs, hb=hb, j=j, c0=c0):
                r = None
                for h in range(8):
                    pH = (psH0, psH1)[h // 4][hb]
                    o_ = pH.t[hs, (h % 4) * 128:(h % 4 + 1) * 128]
                    e.matmul(o_, lhsT=wT[hb].t[hs, h * 64:(h + 1) * 64], rhs=vt3[hs, j, h * 128:(h + 1) * 128],
                             start=(h % 4 == 0), stop=False, skip_group_check=True)
                    o0 = pH.t[64 * hb:64 * hb + 32, (h % 4) * 128:(h % 4 + 1) * 128]
                    r = e.matmul(o0, lhsT=Q3[:, h, c0:c0 + 32], rhs=TB3[:, h, :], start=False, stop=False, skip_group_check=True)
                return r
            P.capture = sg["B"]
            P.op("pe", omm, reads=[wT[hb], vtok, bufQ, TstB[l]], writes=[psH0[hb], psH1[hb]])
            for sub in range(2):
                cc = 2 * pr + sub
                k3 = (k03, k13)[sub]
                khT = (kh0, kh1)[sub][hb]

                def umm(e, hs=hs, j=j, k3=k3):
                    r = None
                    for h in range(8):
                        pc = psAB[h // 4][0]
                        r = e.matmul(pc.t[:, (h % 4) * 128:(h % 4 + 1) * 128], lhsT=k3[:, h, :], rhs=vt3[hs, j, h * 128:(h + 1) * 128],
                                     start=(h % 4 == 0), stop=True, skip_group_check=True)
                    return r
                P.op("pe", umm, reads=[khT, vtok], writes=[psA[0], psA[1], psB[0], psB[1]])
                for q in range(2):
                    pc = psAB[q]
                    P.op("dve", lambda e, q=q, pc=pc: e.tensor_tensor(out=T3[:, 4 * q:4 * q + 4, :], in0=T3[:, 4 * q:4 * q + 4, :],
                                                                      in1=r3(pc[0].t[:, :], 4), op=ALU.add),
                         reads=[Tst[l], pc[0], pc[1]], writes=[Tst[l]])
                for q in range(2):
                    P.op("dve", lambda e, q=q, cc=cc: e.tensor_tensor(
                        out=TB3[:, 4 * q:4 * q + 4, :], in0=T3[:, 4 * q:4 * q + 4, :],
                        in1=bass.AP(FX.t, 68 * q + cc, [[fstep, 128], [17, 4], [0, 128]]), op=ALU.mult),
                        reads=[Tst[l], FX], writes=[TstB[l]])
                for q in range(2):
                    P.op("dve", lambda e, q=q, cc=cc: e.tensor_tensor(
                        out=T3[:, 4 * q:4 * q + 4, :], in0=T3[:, 4 * q:4 * q + 4, :],
                        in1=bass.AP(FX.t, 68 * q + cc, [[fstep, 128], [17, 4], [0, 128]]), op=ALU.mult),
                        reads=[Tst[l], FX], writes=[Tst[l]])
                if sub == 0:
                    qp3 = r3(qp.t[:], 8)

                    def omm2(e, hs=hs, hb=hb, qp3=qp3):
                        r = None
                        for h in range(8):
                            pH = (psH0, psH1)[h // 4][hb]
                            o_ = pH.t[hs, (h % 4) * 128:(h % 4 + 1) * 128]
                            r = e.matmul(o_, lhsT=qp3[:, h, :], rhs=TB3[:, h, :], start=False, stop=True, skip_group_check=True)
                        return r
                    P.op("pe", omm2, reads=[qp, TstB[l]], writes=[psH0[hb], psH1[hb]])
            S_ = sm[hb].t
            P.op("act", lambda e, hs=hs, hb=hb: e.activation(out=E01t[hs, 0:512], in_=psH0[hb].t[hs, :], func=AF.Identity),
                 reads=[psH0[hb]], writes=[E01[hb]])
            P.op("act", lambda e, hs=hs, hb=hb: e.activation(out=E01t[hs, 512:1024], in_=psH1[hb].t[hs, :], func=AF.Identity),
                 reads=[psH1[hb]], writes=[E01[hb]])
            P.capture = sg["C"]
            P.op("act", lambda e, hs=hs, hb=hb: e.activation(out=sqht[hs, :], in_=E01t[hs, :], func=AF.Square),
                 reads=[E01[hb]], writes=[sqh[hb]])
            P.op("dve", lambda e, hs=hs, S_=S_: e.tensor_reduce(out=S_[hs, 16:24], in_=r3(sqht[hs, :], 8), axis=AX.X, op=ALU.add),
                 reads=[sqh[hb]], writes=[sm[hb]])
            P.op("dve", lambda e, hs=hs, S_=S_: e.tensor_scalar(out=S_[hs, 24:32], in0=S_[hs, 16:24], scalar1=1.0 / 128, scalar2=EPS,
                                                                op0=ALU.mult, op1=ALU.add), reads=[sm[hb]], writes=[sm[hb]])
            P.op("act", lambda e, hs=hs, S_=S_: e.activation(out=S_[hs, 40:48], in_=S_[hs, 24:32], func=AF.Ln),
                 reads=[sm[hb]], writes=[sm[hb]])
            P.op("act", lambda e, hs=hs, S_=S_: e.activation(out=S_[hs, 40:48], in_=S_[hs, 40:48], func=AF.Exp, scale=-0.5),
                 reads=[sm[hb]], writes=[sm[hb]])
            P.op("dve", lambda e, hs=hs, S_=S_: e.tensor_tensor(
                out=r3(E01t[hs, :], 8), in0=r3(E01t[hs, :], 8),
                in1=bass.AP(smt, S_[hs, 40:41].offset, [[S_[hs, 0:1].ap[0][0], 64], [1, 8], [0, 128]]),
                op=ALU.mult), reads=[sm[hb]], writes=[E01[hb]])
            for q in range(2):
                cnt["tg"] += 1
                tgt = tg[cnt["tg"] % 2]
                P.op("dve", lambda e, hs=hs, q=q, j=j, tgt=tgt: e.tensor_tensor(
                    out=tgt.t[hs, :], in0=E01t[hs, q * 512:(q + 1) * 512],
                    in1=Gp3[hs, j, q * 512:(q + 1) * 512], op=ALU.mult), reads=[E01[hb], Gp], writes=[tgt])
                P.op("pool", lambda e, hs=hs, q=q, j=j, tgt=tgt: e.tensor_tensor(
                    out=ya3[hs, j, q * 512:(q + 1) * 512], in0=ya3[hs, j, q * 512:(q + 1) * 512], in1=tgt.t[hs, :], op=ALU.add),
                    reads=[yacc, tgt], writes=[yacc])
            P.capture = None
        P.pipeline(hsegs)

        if stop("hg_loop"):
            return
        for j in range(4):
            psXb = psX[0].t[:, :].bitcast(BF16)

            def ytr(e, j=j, psXb=psXb):
                r = None
                for f in range(8):
                    r = e.transpose(psXb[:, f * 128:(f + 1) * 128], ya3[:, j, f * 128:(f + 1) * 128], identB.t[:, :])
                return r
            P.op("pe", ytr, reads=[yacc, identB], writes=[psX[0], psX[1]])
            P.op("act", lambda e, j=j, psXb=psXb: e.activation(out=yT3[:, :, j * 128:(j + 1) * 128], in_=r3(psXb[:, :], 8),
                                                              func=AF.Identity), reads=[psX[0], psX[1]], writes=[yT])
        if stop("ytr"):
            return
        for jo in range(8):
            if jo % 4 == 0:
                sl = next_slab(l, 20 + jo // 4)
                sl3 = sl.t[:, 0:4096].rearrange("p (k c) -> p k c", k=8)
            pst = nextps()
            group_mm(pst, lambda pst=pst: pst[0].t[:, :], lambda k, sl3=sl3, jo=jo: sl3[:, k, (jo % 4) * 128:(jo % 4 + 1) * 128],
                     lambda k: yT3[:, k, :], 8, [yT, sl])
            P.op("dve", lambda e, pst=pst, jo=jo: e.scalar_tensor_tensor(out=xall[:, jo, :], in0=pst[0].t[:, :], scalar=mcol(2, jo),
                                                                         in1=xall[:, jo, :], op0=ALU.mult, op1=ALU.add),
                 reads=[pst[0], pst[1], modT[l], xTc[jo]], writes=[xTc[jo]])
        if dbg and dbg.get("stage") == "mix" and l == dbg.get("nl", DEPTH) - 1:
            for s_ in list(range(22, 33)) + list(range(33, 41)):
                next_slab(l, s_)
            return
        norm(lambda k: hT3[:, k, :], lambda k: hT, lambda k: AT.t[:, (l * 2 + 1) * 8 + k:(l * 2 + 1) * 8 + k + 1],
             lambda k: mcol(3, k), [AT, modT[l]])

        def gchunk(jj):
            if jj < 8:
                return bufQ, Q3[:, jj, :]
            if jj < 16:
                return bufK, K3[:, jj - 8, :]
            return vtok, vtok.t[:, (jj - 16) * 512:(jj - 16 + 1) * 512]
        for s in range(11):
            sl = next_slab(l, 22 + s)
            sl3 = sl.t[:, 0:4096].rearrange("p (k c) -> p k c", k=8)
            for u in range(2):
                jj = 2 * s + u
                res = {}
                for part in range(2):
                    col = part * 256 + u * 128
                    pst = nextps()
                    group_mm(pst, lambda pst=pst: pst[0].t[:, :], lambda k, sl3=sl3, col=col: sl3[:, k, col:col + 128],
                             lambda k: hT3[:, k, :], 8, [hT, sl])
                    cnt["xp"] += 1
                    xpT, accT = xp[cnt["xp"] % 2], acc[cnt["xp"] % 2]
                    cidx = jj + 22 * part
                    conv_chunk(pst, xpT, accT, 3, tailff[l], cidx, f"fcw{l}", f"fcb{l}", cidx, 44, None, l)
                    res[part] = accT
                cnt["tg"] += 1
                sgt = tg[cnt["tg"] % 2]
                P.op("act", lambda e, sgt=sgt, a=res[0]: e.activation(out=sgt.t[:], in_=a.t[:], func=AF.Silu), reads=[res[0]], writes=[sgt])
                gT_, gap = gchunk(jj)
                P.op("dve", lambda e, sgt=sgt, a=res[1], gap=gap: e.tensor_tensor(out=gap, in0=sgt.t[:], in1=a.t[:], op=ALU.mult),
                     reads=[sgt, res[1]], writes=[gT_])
        for jo in range(8):
            sl = next_slab(l, 33 + jo)
            pst = nextps()
            group_mm(pst, lambda pst=pst: pst[0].t[:, :], lambda k, sl=sl: sl.t[:, k * 128:(k + 1) * 128],
                     lambda k: gchunk(k)[1], FFC, [bufQ, bufK, vtok, sl])
            P.op("dve", lambda e, pst=pst, jo=jo: e.scalar_tensor_tensor(out=xall[:, jo, :], in0=pst[0].t[:, :], scalar=mcol(5, jo),
                                                                         in1=xall[:, jo, :], op0=ALU.mult, op1=ALU.add),
                 reads=[pst[0], pst[1], modT[l], xTc[jo]], writes=[xTc[jo]])

    for sq in range(NSEQ):
        for l in range(DEPTH):
            for tl_ in (Cext[l], CextB[l], nF[l], nB[l], Tst[l], TstB[l], tailqk[l], tailff[l], carry[l]):
                P.op("pool", lambda e, tl_=tl_: e.memset(tl_.t[:, :], 0.0), writes=[tl_])
            for w_ in range(2):
                sc_ = lambda k: modT[l].t[:, ((1 + 3 * w_) * 8) * NSEQ + sq: ((1 + 3 * w_) * 8 + 8) * NSEQ + sq]
                P.op("dve", lambda e, l=l, w_=w_, sq=sq: e.scalar_tensor_tensor(
                    out=AT.t[:, (l * 2 + w_) * 8:(l * 2 + w_) * 8 + 8],
                    in0=bass.AP(modT[l].t, modT[l].t[:, ((1 + 3 * w_) * 8) * NSEQ + sq:((1 + 3 * w_) * 8) * NSEQ + sq + 1].offset,
                                [[modT[l].t[:, 0:1].ap[0][0], 128], [NSEQ, 8]]),
                    scalar=1.0, in1=vcol(f"mixw{l}" if w_ == 0 else f"ffnw{l}", 0, 8), op0=ALU.add, op1=ALU.mult),
                    reads=[modT[l], V], writes=[AT])
        for ti in range(NT):
            P.dma("sp", "xin", lambda e, sq=sq, ti=ti: [e.dma_start(out=xall, in_=xin[sq, :, :, ti * TT:(ti + 1) * TT])],
                  writes=xTc)
            for l in range(DEPTH):
                if dbg and l >= dbg.get("nl", DEPTH):
                    for s_ in order:
                        next_slab(l, s_)
                    continue
                layer(l, sq)
            if not (dbg and not dbg.get("final", True)):
                norm(lambda k: xall[:, k, :], lambda k: xTc[k], lambda k: vcol("finw", k), lambda k: 0.0, [V])
            P.dma("sp", "xout", lambda e, sq=sq, ti=ti: [e.dma_start(out=out[sq, :, :, ti * TT:(ti + 1) * TT], in_=xall)],
                  reads=xTc)
    P.finish()
    print("ops:", len(P.ops), "engine signal counts:", P.counts, "sems:", P.nsem)
    return nc


N_CORES = 8


def kernel(x, c, ada_w, ada_b, mix_norm_w, in_w, in_b, mlstm_conv_w, mlstm_conv_b,
           mlstm_norm_w, hgrn_lower_bounds, hgrn_norm_w, out_w, ffn_norm_w, ffn_up_w,
           ffn_conv_w, ffn_conv_b, ffn_down_w, final_norm_w):
    inp = dict(x=x, c=c, ada_w=ada_w, ada_b=ada_b, mix_norm_w=mix_norm_w, in_w=in_w, in_b=in_b,
               mlstm_conv_w=mlstm_conv_w, mlstm_conv_b=mlstm_conv_b, mlstm_norm_w=mlstm_norm_w,
               hgrn_lower_bounds=hgrn_lower_bounds, hgrn_norm_w=hgrn_norm_w, out_w=out_w,
               ffn_norm_w=ffn_norm_w, ffn_up_w=ffn_up_w, ffn_conv_w=ffn_conv_w, ffn_conv_b=ffn_conv_b,
               ffn_down_w=ffn_down_w, final_norm_w=final_norm_w)
    inp = {k: np.asarray(v, dtype=np.float32) for k, v in inp.items()}
    B, S, _ = inp["x"].shape
    assert B % N_CORES == 0
    nseq = B // N_CORES
    shared = prep_shared(inp)
    nc = build_nc(nseq, S)
    in_maps = []
    for i in range(N_CORES):
        core = prep_core(inp["x"][i * nseq:(i + 1) * nseq], inp["c"][i * nseq:(i + 1) * nseq])
        in_maps.append({**shared, **core})
    res = run_bass_kernel_spmd(nc, in_maps, core_ids=list(range(N_CORES)))
    outs = [unprep_out(np.asarray(r["oT"])) for r in res.results]
    return np.concatenate(outs, axis=0).astype(np.float32)
```

```python
import numpy as np
import concourse.bass as bass
import concourse.mybir as mybir
from concourse.bass_utils import run_bass_kernel_spmd

F32 = mybir.dt.float32
BF16 = mybir.dt.bfloat16
AF = mybir.ActivationFunctionType
ALU = mybir.AluOpType
AX = mybir.AxisListType


class T:
    __slots__ = ("name", "t", "lw", "rd", "psum")

    def __init__(self, name, t, psum=False):
        self.name = name
        self.t = t
        self.lw = None
        self.rd = []
        self.psum = psum


class Op:
    __slots__ = ("eng", "fn", "reads", "writes", "chan", "deps", "sig", "ndma", "idx", "need_sig")

    def __init__(self, eng, fn, reads, writes, chan=None):
        self.eng = eng
        self.fn = fn
        self.reads = reads
        self.writes = writes
        self.chan = chan
        self.deps = []
        self.sig = None
        self.ndma = 0
        self.need_sig = False


class Prog:
    def __init__(self, nc):
        self.nc = nc
        self.ops = []
        self.chan_last = {}

    capture = None

    def op(self, eng, fn, reads=(), writes=()):
        o = Op(eng, fn, list(reads), list(writes))
        if self.capture is not None:
            self.capture.append(o)
        else:
            self._add(o)
        return o

    def replay(self, lst):
        for o in lst:
            self._add(o)

    def pipeline(self, segs):
        import os
        if os.environ.get("NOPIPE"):
            for sg in segs:
                self.replay(sg["A"]); self.replay(sg["B"]); self.replay(sg["C"])
            return
        if segs:
            self.replay(segs[0]["A"])
        for i, sg in enumerate(segs):
            if i + 1 < len(segs):
                self.replay(segs[i + 1]["A"])
            self.replay(sg["B"])
            self.replay(sg["C"])

    def dma(self, eng, chan, fn, reads=(), writes=()):
        o = Op(eng, fn, list(reads), list(writes), chan=chan)
        self._add(o)
        return o

    def _add(self, o):
        deps = []
        for t in o.reads:
            if t.lw is not None:
                deps.append(t.lw)
            if t.psum:
                deps.extend(r for r in t.rd if r.eng != o.eng)
        for t in o.writes:
            if t.lw is not None:
                deps.append(t.lw)
            deps.extend(t.rd)
        if o.chan is not None:
            p = self.chan_last.get(o.chan)
            if p is not None:
                deps.append(p)
            self.chan_last[o.chan] = o
        for t in o.reads:
            t.rd.append(o)
        for t in o.writes:
            t.lw = o
            t.rd = []
        seen = set()
        for d in deps:
            if id(d) in seen or d is o:
                continue
            seen.add(id(d))
            if d.chan is None and o.chan is None and d.eng == "pe" and o.eng == "pe":
                continue
            o.deps.append(d)
            d.need_sig = True
        o.idx = len(self.ops)
        self.ops.append(o)

    def finish(self):
        nc = self.nc
        engs = {"pe": nc.tensor, "act": nc.scalar, "dve": nc.vector, "pool": nc.gpsimd, "sp": nc.sync}
        fin = Op("sp", None, [], [])
        fin.deps = [o for o in self.chan_last.values()]
        fin.idx = len(self.ops)
        self.ops.append(fin)
        esem = {k: nc.alloc_semaphore("s_" + k) for k in engs}
        csem = {}
        ccount = {}
        ecount = {k: 0 for k in engs}
        for o in self.ops:
            if o.chan is not None:
                if o.chan not in csem:
                    csem[o.chan] = nc.alloc_semaphore("c_" + o.chan)
                    ccount[o.chan] = 0
                n = getattr(o.fn, "ndma", 1)
                ccount[o.chan] += 16 * n
                o.sig = (csem[o.chan], ccount[o.chan])
            elif o.need_sig:
                ecount[o.eng] += 1
                o.sig = (esem[o.eng], ecount[o.eng])
        self.nsem = len(esem) + len(csem)
        by_eng = {k: [] for k in engs}
        for o in self.ops:
            by_eng[o.eng].append(o)

        def emit(name, e):
            waited = {}
            for o in by_eng[name]:
                for d in o.deps:
                    sem, val = d.sig
                    key = id(sem)
                    if waited.get(key, 0) >= val:
                        continue
                    waited[key] = val
                    e.wait_ge(sem, val)
                if o.fn is None:
                    continue
                r = o.fn(e)
                if o.chan is not None:
                    assert len(r) == getattr(o.fn, "ndma", 1), (len(r), o.chan)
                    for ins in r:
                        ins.then_inc(o.sig[0], 16)
                elif o.sig is not None:
                    r.then_inc(o.sig[0], 1)

        with nc.Block() as block:
            @block.tensor
            def _(e):
                emit("pe", e)

            @block.scalar
            def _(e):
                emit("act", e)

            @block.vector
            def _(e):
                emit("dve", e)

            @block.gpsimd
            def _(e):
                emit("pool", e)

            @block.sync
            def _(e):
                emit("sp", e)
        self.counts = ecount


D = 1024
KC = 8
TT = 512
HEADS = 8
PW = 10256
FF = 2816
FFC = 22
DEPTH = 2
NSLAB = 42
SLABW = 9 * 512
EPS = 1e-6
CAP = 15.0
LNSCALE = float(np.log(128.0 ** -0.5))
MASKNEG = -10000.0
C_QK, C_V, C_O, C_I, C_F, C_HQ, C_HF, C_HI, C_HG, C_GA, C_GB = (
    0, 2048, 3072, 4096, 4104, 4112, 5136, 6160, 7184, 8208, 9232)


def vec_layout():
    off = {}
    n = 0

    def add(name, w):
        nonlocal n
        off[name] = n
        n += w
    for l in range(DEPTH):
        add(f"mixw{l}", 8); add(f"ffnw{l}", 8); add(f"inbqk{l}", 16); add(f"inbhq{l}", 8)
        add(f"inbhf{l}", 8); add(f"cw{l}", 64); add(f"cb{l}", 16); add(f"lbraw{l}", 8)
        add(f"fcw{l}", 132); add(f"fcb{l}", 44); add(f"adab{l}", 48); add(f"inbi{l}", 1); add(f"inbf{l}", 1)
    add("finw", 8)
    return off, n


def _fm(v):
    v = np.asarray(v, np.float32)
    return np.ascontiguousarray(v.reshape(-1, 128).T)


def _slab8(w):
    return np.ascontiguousarray(w.reshape(8, 128, 512).transpose(1, 0, 2)).reshape(128, 4096)


def prep_shared(inp):
    off, nv = vec_layout()
    V = np.zeros((128, nv), np.float32)
    wsl = np.zeros((DEPTH, NSLAB, 128, SLABW), np.float32)
    adaw = np.zeros((DEPTH, 48, 128, 8 * 128), np.float32)
    nrm = np.zeros((DEPTH, 2, D), np.float32)
    for l in range(DEPTH):
        inb = inp["in_b"][l]
        V[:, off[f"mixw{l}"]:off[f"mixw{l}"] + 8] = _fm(inp["mix_norm_w"][l])
        V[:, off[f"ffnw{l}"]:off[f"ffnw{l}"] + 8] = _fm(inp["ffn_norm_w"][l])
        V[:, off[f"inbqk{l}"]:off[f"inbqk{l}"] + 16] = _fm(inb[C_QK:C_QK + 2048])
        V[:, off[f"inbhq{l}"]:off[f"inbhq{l}"] + 8] = _fm(inb[C_HQ:C_HQ + 1024])
        V[:, off[f"inbhf{l}"]:off[f"inbhf{l}"] + 8] = _fm(inb[C_HF:C_HF + 1024])
        for j in range(4):
            V[:, off[f"cw{l}"] + 16 * j:off[f"cw{l}"] + 16 * j + 16] = _fm(inp["mlstm_conv_w"][l, j])
        V[:, off[f"cb{l}"]:off[f"cb{l}"] + 16] = _fm(inp["mlstm_conv_b"][l])
        V[:, off[f"lbraw{l}"]:off[f"lbraw{l}"] + 8] = _fm(inp["hgrn_lower_bounds"][l])
        for j in range(3):
            V[:, off[f"fcw{l}"] + 44 * j:off[f"fcw{l}"] + 44 * j + 44] = _fm(inp["ffn_conv_w"][l, j])
        V[:, off[f"fcb{l}"]:off[f"fcb{l}"] + 44] = _fm(inp["ffn_conv_b"][l])
        V[:, off[f"adab{l}"]:off[f"adab{l}"] + 48] = _fm(inp["ada_b"][l])
        V[0:8, off[f"inbi{l}"]] = inb[C_I:C_I + 8]
        V[0:8, off[f"inbf{l}"]] = inb[C_F:C_F + 8]
        W = inp["in_w"][l]
        fm_cols = [C_QK, C_QK + 512, C_QK + 1024, C_QK + 1536, C_HQ, C_HQ + 512, C_HF, C_HF + 512]
        for s, c0 in enumerate(fm_cols):
            wsl[l, s, :, :4096] = _slab8(W[:, c0:c0 + 512])
        tm_cols = [C_V, C_V + 512, C_O, C_O + 512, C_GA, C_GA + 512, C_HI, C_HI + 512,
                   C_HG, C_HG + 512, C_GB, C_GB + 512]
        for i, c0 in enumerate(tm_cols):
            wsl[l, 8 + i, :, :4096] = _slab8(W[:, c0:c0 + 512])
            wsl[l, 8 + i, 0, 4096:4608] = inb[c0:c0 + 512]
        Wo = inp["out_w"][l]
        wsl[l, 20, :, :4096] = _slab8(Wo[:, 0:512])
        wsl[l, 21, :, :4096] = _slab8(Wo[:, 512:1024])
        Wu = inp["ffn_up_w"][l]
        for s in range(11):
            cols = np.concatenate([np.arange(256 * s, 256 * s + 256), FF + np.arange(256 * s, 256 * s + 256)])
            wsl[l, 22 + s, :, :4096] = _slab8(Wu[:, cols])
        Wd = inp["ffn_down_w"][l]
        for jo in range(8):
            blk = Wd[:, jo * 128:(jo + 1) * 128].reshape(FFC, 128, 128).transpose(1, 0, 2)
            wsl[l, 33 + jo, :, :FFC * 128] = blk.reshape(128, FFC * 128)
        wif = W[:, C_I:C_I + 16].reshape(8, 128, 16).transpose(1, 0, 2)
        wsl[l, 41, :, :128] = wif.reshape(128, 128)
        Wa = inp["ada_w"][l]
        for j in range(48):
            adaw[l, j] = Wa[:, j * 128:(j + 1) * 128].reshape(8, 128, 128).transpose(1, 0, 2).reshape(128, 1024)
        nrm[l, 0] = inp["mlstm_norm_w"][l]
        nrm[l, 1] = inp["hgrn_norm_w"][l]
    V[:, off["finw"]:off["finw"] + 8] = _fm(inp["final_norm_w"])
    return {"vecs": V, "wsl": wsl, "adaw": adaw, "nrm": nrm}


def prep_core(x, c):
    nseq, S, _ = x.shape
    xT = np.ascontiguousarray(x.reshape(nseq, S, 8, 128).transpose(0, 3, 2, 1))
    cT = np.ascontiguousarray(c.reshape(nseq, 8, 128).transpose(2, 1, 0))
    return {"xT": xT, "cT": cT}


def unprep_out(oT):
    nseq, _, _, S = oT.shape
    return np.ascontiguousarray(oT.transpose(0, 3, 2, 1)).reshape(nseq, S, D)


def build_nc(NSEQ, S, NSLOT=2, dbg=None):
    assert S % TT == 0
    NT = S // TT
    off, NV = vec_layout()
    nc = bass.Bass("TRN2", target_bir_lowering=False)
    P = Prog(nc)

    xin = nc.dram_tensor("xT", [NSEQ, 128, 8, S], F32, kind="ExternalInput")
    cin = nc.dram_tensor("cT", [128, 8, NSEQ], F32, kind="ExternalInput")
    vin = nc.dram_tensor("vecs", [128, NV], F32, kind="ExternalInput")
    wsl = nc.dram_tensor("wsl", [DEPTH, NSLAB, 128, SLABW], F32, kind="ExternalInput")
    adw = nc.dram_tensor("adaw", [DEPTH, 48, 128, 1024], F32, kind="ExternalInput")
    nrm = nc.dram_tensor("nrm", [DEPTH, 2, D], F32, kind="ExternalInput")
    out = nc.dram_tensor("oT", [NSEQ, 128, 8, S], F32, kind="ExternalOutput")
    scr = nc.dram_tensor("scr", [DEPTH, NSLAB, 128, SLABW], BF16, kind="Internal")
    scrT = [[T(f"scr{l}_{s}", None) for s in range(NSLAB)] for l in range(DEPTH)]

    tot = [0]

    def sb(name, n, dt=F32):
        t = nc.alloc_sbuf_tensor(name, [128, n], dt)
        tot[0] += n * (4 if dt == F32 else 2)
        return T(name, t)

    def ps(name):
        t = nc.alloc_psum_tensor(name, [128, 512], F32)
        tt = T(name, t, True)
        return (tt, tt)

    def r3(ap, a):
        return ap.rearrange("p (a b) -> p a b", a=a)

    def bcast_mid(tl, p0, np_, offs, n_outer, outer_step, n_inner):
        base = tl.t[p0:p0 + np_, 0:1]
        pstep = base.ap[0][0]
        return bass.AP(tl.t, base.offset + offs, [[pstep, np_], [outer_step, n_outer], [0, n_inner]])

    V = sb("V", NV)
    identF = sb("identF", 128)
    identB = sb("identB", 128, BF16)
    onesB = sb("onesB", 128, BF16)
    negonesF = sb("negonesF", 128)
    lhsD = sb("lhsD", 512)
    rhsD = [sb(f"rhsD{i}", 512) for i in range(2)]
    negbm8 = sb("negbm8", 512)
    mask01 = sb("mask01", 512, BF16)
    negh = sb("negh", 8)
    ones1 = sb("ones1", 1)
    modT = [sb(f"modT{l}", 48 * NSEQ) for l in range(DEPTH)]
    AT = sb("AT", DEPTH * 2 * 8)
    LB = sb("LB", DEPTH * 8)
    OML = sb("OML", DEPTH * 8)
    NOML = sb("NOML", DEPTH * 8)
    gb15 = sb("gb15", DEPTH * 2)
    cs = sb("cs", 8 * NSEQ)
    Cext = [sb(f"Cext{l}", 1024) for l in range(DEPTH)]
    CextB = [sb(f"CextB{l}", 1024, BF16) for l in range(DEPTH)]
    nF = [sb(f"nF{l}", 8) for l in range(DEPTH)]
    nB = [sb(f"nB{l}", 8, BF16) for l in range(DEPTH)]
    Tst = [sb(f"Tst{l}", 1024) for l in range(DEPTH)]
    TstB = [sb(f"TstB{l}", 1024, BF16) for l in range(DEPTH)]
    tailqk = [sb(f"tailqk{l}", 16 * 3) for l in range(DEPTH)]
    tailff = [sb(f"tailff{l}", 44 * 2) for l in range(DEPTH)]
    carry = [sb(f"carry{l}", 2) for l in range(DEPTH)]
    xT_t = nc.alloc_sbuf_tensor("xT_sb", [128, 8 * 512], F32)
    tot[0] += 16384
    xTc = [T(f"xT{k}", xT_t) for k in range(8)]
    hT = sb("hT", 4096, BF16)
    yT = sb("yT", 4096, BF16)
    slabs = [sb(f"slab{i}", SLABW, BF16) for i in range(NSLOT)]
    bufQ = sb("bufQ", 4096, BF16)
    bufK = sb("bufK", 4096, BF16)
    xp = [sb(f"xp{i}", 516) for i in range(2)]
    acc = [sb(f"acc{i}", 512) for i in range(2)]
    GA = sb("GA", 512); GF = sb("GF", 512); GF2 = sb("GF2", 512)
    Bcat = sb("Bcat", 516); ucat = sb("ucat", 516)
    ARG = sb("ARG", 512); GE = sb("GE", 512)
    GT = sb("GT", 4 * 72)
    wibd = [sb(f"wibd{i}", 512) for i in range(2)]
    dec = [sb(f"dec{i}", 8) for i in range(2)]
    vtok = sb("vtok", 4096, BF16)
    Gp = sb("Gp", 4096, BF16)
    yacc = sb("yacc", 4096, BF16)
    tg = [sb(f"tg{i}", 512, BF16) for i in range(2)]
    nw = sb("nw", 1024)
    DTt = nc.alloc_sbuf_tensor("DT", [128, 512], F32); tot[0] += 2048
    DT = (T("DT_lo", DTt), T("DT_hi", DTt))
    wTt = nc.alloc_sbuf_tensor("wT", [128, 512], BF16); tot[0] += 1024
    wT = (T("wT_lo", wTt), T("wT_hi", wTt))
    qsc = [sb(f"qsc{i}", 512, BF16) for i in range(2)]
    kht = [nc.alloc_sbuf_tensor(f"khat{i}", [128, 1024], BF16) for i in range(2)]
    tot[0] += 4096
    khat = [(T(f"khat{i}_lo", kht[i]), T(f"khat{i}_hi", kht[i])) for i in range(2)]
    sqht = nc.alloc_sbuf_tensor("sqh", [128, 1024], F32); tot[0] += 4096
    sqh = (T("sqh_lo", sqht), T("sqh_hi", sqht))
    E01t = nc.alloc_sbuf_tensor("E01", [128, 1024], F32); tot[0] += 4096
    E01 = (T("E01_lo", E01t), T("E01_hi", E01t))
    smt = nc.alloc_sbuf_tensor("sm", [128, 64], F32); tot[0] += 256
    sm = (T("sm_lo", smt), T("sm_hi", smt))
    tmpF = [sb(f"tmpF{i}", 512) for i in range(4)]
    kkb = sb("kkb", 512, BF16); epb = sb("epb", 512, BF16); enb = sb("enb", 512, BF16)
    FA = sb("FA", 136); FX = sb("FX", 136)
    Qpad = [sb(f"Qpad{i}", 512, BF16) for i in range(2)]
    rm = sb("rm", 2)
    cbq = sb("cbq", DEPTH * 16)
    print("SBUF bytes/partition:", tot[0], "remaining", nc.sbuf_bytes_remaining)

    psA = ps("psA"); psB = ps("psB"); psS = ps("psS"); psD = ps("psD")
    psWI = ps("psWI"); psH0 = ps("psH0"); psH1 = ps("psH1"); psX = ps("psX")
    psAB = [psA, psB]

    def HS(hb):
        return slice(64 * hb, 64 * hb + 64)

    def vcol(name, j=0, n=1, p=128):
        return V.t[0:p, off[name] + j:off[name] + j + n]

    P.dma("sp", "misc", lambda e: [e.dma_start(out=V.t[:], in_=vin[:, :])], writes=[V])
    P.dma("sp", "misc", lambda e: [e.dma_start(out=r3(cs.t[:], 8), in_=cin[:, :, :])], writes=[cs])
    pool = "pool"
    P.op(pool, lambda e: e.memset(identF.t[:], 1.0), writes=[identF])
    P.op(pool, lambda e: e.affine_select(out=identF.t[:], in_=identF.t[:], pattern=[[-1, 128]],
                                         compare_op=ALU.is_equal, fill=0.0, base=0, channel_multiplier=1),
         reads=[identF], writes=[identF])
    P.op(pool, lambda e: e.tensor_copy(out=identB.t[:], in_=identF.t[:]), reads=[identF], writes=[identB])
    P.op(pool, lambda e: e.memset(onesB.t[:], 1.0), writes=[onesB])
    P.op(pool, lambda e: e.memset(negonesF.t[:], -1.0), writes=[negonesF])
    P.op(pool, lambda e: e.memset(negh.t[:], -0.5), writes=[negh])
    P.op(pool, lambda e: e.memset(ones1.t[:], 1.0), writes=[ones1])
    P.op(pool, lambda e: e.memset(lhsD.t[:], 0.0), writes=[lhsD])
    P.op(pool, lambda e: e.memset(lhsD.t[0:8, :], 1.0), writes=[lhsD])
    for c in range(8):
        P.op(pool, lambda e, c=c: e.tensor_copy(out=lhsD.t[64:128, c * 64:(c + 1) * 64], in_=identF.t[64:128, 64:128]),
             reads=[identF], writes=[lhsD])
    P.op(pool, lambda e: e.memset(negbm8.t[:], -1.0), writes=[negbm8])
    P.op(pool, lambda e: e.affine_select(out=r3(negbm8.t[0:8, :], 8), in_=r3(negbm8.t[0:8, :], 8),
                                         pattern=[[-1, 8], [0, 64]], compare_op=ALU.is_equal, fill=0.0,
                                         base=0, channel_multiplier=1), reads=[negbm8], writes=[negbm8])
    mb = tmpF[0]
    P.op(pool, lambda e: e.memset(mb.t[:], LNSCALE), writes=[mb])
    P.op(pool, lambda e: e.affine_select(out=r3(mb.t[0:64, :], 8), in_=r3(mb.t[0:64, :], 8),
                                         pattern=[[0, 8], [1, 64]], compare_op=ALU.is_ge, fill=MASKNEG,
                                         base=0, channel_multiplier=-1), reads=[mb], writes=[mb])
    m1 = tmpF[1]
    P.op(pool, lambda e: e.memset(m1.t[:], 1.0), writes=[m1])
    P.op(pool, lambda e: e.affine_select(out=r3(m1.t[0:64, :], 8), in_=r3(m1.t[0:64, :], 8),
                                         pattern=[[0, 8], [1, 64]], compare_op=ALU.is_ge, fill=0.0,
                                         base=0, channel_multiplier=-1), reads=[m1], writes=[m1])
    P.op(pool, lambda e: e.memset(r3(m1.t[0:32, :], 8)[:, :, 32:64], 0.0), reads=[m1], writes=[m1])
    P.op(pool, lambda e: e.memset(rm.t[:, 0:1], 1.0), writes=[rm])
    P.op(pool, lambda e: e.memset(rm.t[:, 1:2], 0.0), writes=[rm])
    for b0 in (32, 96):
        P.op(pool, lambda e, b0=b0: e.memset(rm.t[b0:b0 + 32, 0:1], 0.0), writes=[rm])
        P.op(pool, lambda e, b0=b0: e.memset(rm.t[b0:b0 + 32, 1:2], 1.0), writes=[rm])
    for i in range(2):
        P.op(pool, lambda e, i=i: e.memset(Qpad[i].t[:, :], 0.0), writes=[Qpad[i]])
    P.op(pool, lambda e: e.tensor_copy(out=mask01.t[0:64, :], in_=m1.t[0:64, :]), reads=[m1], writes=[mask01])
    P.op(pool, lambda e: e.tensor_copy(out=mask01.t[64:128, :], in_=m1.t[0:64, :]), reads=[m1], writes=[mask01])
    for i in range(2):
        P.op(pool, lambda e, i=i: e.memset(rhsD[i].t[:], 0.0), writes=[rhsD[i]])
        P.op(pool, lambda e, i=i: e.tensor_scalar(out=rhsD[i].t[32:40, :], in0=negbm8.t[0:8, :], scalar1=-1.0,
                                                  scalar2=None, op0=ALU.mult), reads=[negbm8], writes=[rhsD[i]])
        P.op(pool, lambda e, i=i: e.tensor_copy(out=rhsD[i].t[64:128, :], in_=mb.t[0:64, :]), reads=[mb], writes=[rhsD[i]])
    P.op(pool, lambda e: e.memset(ARG.t[:], 0.0), writes=[ARG])
    P.op("dve", lambda e: e.tensor_tensor(out=LB.t[:, 8:16], in0=vcol("lbraw1", 0, 8), in1=vcol("lbraw0", 0, 8),
                                          op=ALU.subtract), reads=[V], writes=[LB])
    P.op("act", lambda e: e.activation(out=LB.t[:, 8:16], in_=LB.t[:, 8:16], func=AF.Sigmoid), reads=[LB], writes=[LB])
    P.op("dve", lambda e: e.memset(LB.t[:, 0:8], 0.0), reads=[LB], writes=[LB])
    P.op("dve", lambda e: e.tensor_scalar(out=OML.t[:], in0=LB.t[:], scalar1=-1.0, scalar2=1.0, op0=ALU.mult, op1=ALU.add),
         reads=[LB], writes=[OML])
    P.op("dve", lambda e: e.tensor_scalar(out=NOML.t[:], in0=OML.t[:], scalar1=-1.0, scalar2=None, op0=ALU.mult),
         reads=[OML], writes=[NOML])
    for l in range(DEPTH):
        P.op("dve", lambda e, l=l: e.tensor_scalar(out=gb15.t[0:8, 2 * l:2 * l + 1], in0=vcol(f"inbi{l}", 0, 1, 8),
                                                   scalar1=1.0 / CAP, scalar2=None, op0=ALU.mult), reads=[V], writes=[gb15])
        P.op("dve", lambda e, l=l: e.tensor_scalar(out=gb15.t[0:8, 2 * l + 1:2 * l + 2], in0=vcol(f"inbf{l}", 0, 1, 8),
                                                   scalar1=1.0 / CAP, scalar2=None, op0=ALU.mult), reads=[V], writes=[gb15])
    P.op("act", lambda e: e.activation(out=cs.t[:], in_=cs.t[:], func=AF.Silu), reads=[cs], writes=[cs])
    for l in range(DEPTH):
        P.op("dve", lambda e, l=l: e.tensor_tensor(out=cbq.t[:, l * 16:(l + 1) * 16], in0=vcol(f"inbqk{l}", 0, 16),
                                                   in1=V.t[:, off[f"cw{l}"] + 48:off[f"cw{l}"] + 64], op=ALU.mult), reads=[V], writes=[cbq])
        P.op("dve", lambda e, l=l: e.tensor_tensor(out=cbq.t[:, l * 16:(l + 1) * 16], in0=cbq.t[:, l * 16:(l + 1) * 16],
                                                   in1=vcol(f"cb{l}", 0, 16), op=ALU.add), reads=[V, cbq], writes=[cbq])

    for l in range(DEPTH):
        for s in range(NSLAB):
            P.dma("pool", f"cast{(l * NSLAB + s) % 6}",
                  lambda e, l=l, s=s: [e.dma_start(out=scr[l, s, :, :], in_=wsl[l, s, :, :])], writes=[scrT[l][s]])

    AW = [T("AW0", sqht), T("AW1", E01t)]
    for l in range(DEPTH):
        for j in range(48):
            a = AW[j % 2]
            P.dma("sp", f"aw{j % 2}", lambda e, l=l, j=j, a=a: [e.dma_start(out=a.t[:, :], in_=adw[l, j, :, :])],
                  writes=[a, sqh[0], sqh[1]] if j % 2 == 0 else [a, E01[0], E01[1]])

            def mm(e, l=l, j=j, a=a):
                r = None
                for k in range(8):
                    r = e.matmul(psA[0].t[:, j * NSEQ:(j + 1) * NSEQ], lhsT=a.t[:, k * 128:(k + 1) * 128],
                                 rhs=r3(cs.t[:], 8)[:, k, :], start=(j == 0 and k == 0), stop=(k == 7),
                                 skip_group_check=True)
                return r
            P.op("pe", mm, reads=[a, cs], writes=[psA[0], psA[1]])
        P.op("dve", lambda e, l=l: e.tensor_tensor(
            out=r3(modT[l].t[:], 48), in0=r3(psA[0].t[:, 0:48 * NSEQ], 48),
            in1=bcast_mid(V, 0, 128, off[f"adab{l}"], 48, 1, NSEQ), op=ALU.add),
            reads=[psA[0], psA[1], V], writes=[modT[l]])

    order = [0, 1, 2, 3, 41, 8, 9, 10, 11, 12, 13, 4, 5, 6, 7, 14, 15, 16, 17, 18, 19, 20, 21] + \
        list(range(22, 33)) + list(range(33, 41))
    width = {s: 4096 for s in range(NSLAB)}
    for s in range(8, 20):
        width[s] = 4608
    for s in range(33, 41):
        width[s] = FFC * 128
    width[41] = 128
    reqs = []
    for _sq in range(NSEQ):
        for _ti in range(NT):
            for l in range(DEPTH):
                for s in order:
                    reqs.append((l, s))
    rq = {"issued": 0, "next": 0}

    def issue_until(n):
        while rq["issued"] < min(n, len(reqs)):
            i = rq["issued"]
            l, s = reqs[i]
            sl = slabs[i % NSLOT]
            w = width[s]
            P.dma("sp", f"slab{i % NSLOT}",
                  lambda e, l=l, s=s, sl=sl, w=w: [e.dma_start(out=sl.t[:, 0:w], in_=scr[l, s, :, 0:w])],
                  reads=[scrT[l][s]], writes=[sl])
            rq["issued"] += 1

    def next_slab(l, s):
        i = rq["next"]
        assert reqs[i] == (l, s), (reqs[i], l, s)
        issue_until(i + NSLOT)
        rq["next"] += 1
        return slabs[i % NSLOT]

    cnt = {"ps": 0, "xp": 0, "tg": 0, "c": 0}

    psROT = [psA, psB, psS, psD, psH0, psH1]

    def nextps():
        cnt["ps"] += 1
        return psROT[cnt["ps"] % len(psROT)]

    rs, rstd = tmpF[0], tmpF[1]
    tmpk = [tmpF[2], tmpF[3]]

    def norm(dst_fn, dst_T, scale_fn, bias_fn, extra_reads):
        xall = r3(xT_t[:, :], 8)
        P.op("act", lambda e: e.activation(out=yT.t[:], in_=xT_t[:, :], func=AF.Square), reads=xTc, writes=[yT])

        def mm(e):
            r = None
            for k in range(8):
                r = e.matmul(psA[0].t[:, :], lhsT=onesB.t[:, :], rhs=yT.t[:, k * 512:(k + 1) * 512],
                             start=(k == 0), stop=(k == 7))
            return r
        P.op("pe", mm, reads=[onesB, yT], writes=[psA[0], psA[1]])
        P.op("dve", lambda e: e.tensor_scalar(out=rs.t[:], in0=psA[0].t[:, :], scalar1=1.0 / D, scalar2=EPS,
                                              op0=ALU.mult, op1=ALU.add), reads=[psA[0], psA[1]], writes=[rs])
        P.op("act", lambda e: e.activation(out=rstd.t[:], in_=rs.t[:], func=AF.Ln), reads=[rs], writes=[rstd])
        P.op("act", lambda e: e.activation(out=rstd.t[:], in_=rstd.t[:], func=AF.Exp, scale=-0.5), reads=[rstd], writes=[rstd])
        for k in range(8):
            tk = tmpk[k % 2]
            P.op("dve", lambda e, k=k, tk=tk: e.tensor_tensor(out=tk.t[:], in0=xall[:, k, :], in1=rstd.t[:], op=ALU.mult),
                 reads=[xTc[k], rstd], writes=[tk])
            P.op("act", lambda e, k=k, tk=tk: e.activation(out=dst_fn(k), in_=tk.t[:], func=AF.Identity,
                                                          scale=scale_fn(k), bias=bias_fn(k)),
                 reads=[tk] + extra_reads, writes=[dst_T(k)])

    def group_mm(pst, out_ap_fn, lhs_fn, rhs_fn, nk, reads, extra=None):
        def mm(e):
            r = None
            for k in range(nk):
                r = e.matmul(out_ap_fn(), lhsT=lhs_fn(k), rhs=rhs_fn(k), start=(k == 0),
                             stop=(k == nk - 1 and extra is None))
            if extra is not None:
                r = e.matmul(out_ap_fn(), lhsT=extra[0], rhs=extra[1], start=False, stop=True)
            return r
        P.op("pe", mm, reads=reads, writes=[pst[0], pst[1]])

    hT3 = r3(hT.t[:], 8)
    yT3 = r3(yT.t[:], 8)
    Q3 = r3(bufQ.t[:], 8)
    K3 = r3(bufK.t[:], 8)
    xall = r3(xT_t[:, :], 8)
    vt3 = r3(vtok.t[:], 4)
    Gp3 = r3(Gp.t[:], 4)
    ya3 = r3(yacc.t[:], 4)

    def tm_slab(l, s, evac):
        sl = next_slab(l, s)
        sl3 = sl.t[:, 0:4096].rearrange("p (k c) -> p k c", k=8)
        for j in range(4):
            pst = nextps()
            group_mm(pst, lambda pst=pst: pst[0].t[:, :], lambda k, j=j: hT3[:, k, j * 128:(j + 1) * 128],
                     lambda k, sl3=sl3: sl3[:, k, :], 8, [hT, sl],
                     extra=(onesB.t[0:1, 0:128], sl.t[0:1, 4096:4608]))
            evac(j, pst)

    def conv_chunk(pst, xpT, accT, ntap, tailT, tidx, wname, bname, cidx, nch, bias_ap, l):
        nt = ntap - 1
        P.op("pool", lambda e: e.tensor_copy(out=xpT.t[:, 0:nt], in_=tailT.t[:, tidx * nt:(tidx + 1) * nt]),
             reads=[tailT], writes=[xpT])
        if bias_ap is None:
            P.op("act", lambda e: e.activation(out=xpT.t[:, nt:nt + 512], in_=pst[0].t[:, :], func=AF.Identity),
                 reads=[pst[0], pst[1]], writes=[xpT])
        else:
            P.op("act", lambda e: e.activation(out=xpT.t[:, nt:nt + 512], in_=pst[0].t[:, :], func=AF.Identity, bias=bias_ap),
                 reads=[pst[0], pst[1], V], writes=[xpT])
        P.op("pool", lambda e: e.tensor_copy(out=tailT.t[:, tidx * nt:(tidx + 1) * nt], in_=xpT.t[:, 512:512 + nt]),
             reads=[xpT], writes=[tailT])
        wcol = lambda j: V.t[:, off[wname] + nch * j + cidx:off[wname] + nch * j + cidx + 1]
        bcol = V.t[:, off[bname] + cidx:off[bname] + cidx + 1]
        if bias_ap is not None:
            bcol = cbq.t[:, l * 16 + cidx:l * 16 + cidx + 1]
        P.op("act", lambda e: e.activation(out=accT.t[:], in_=pst[0].t[:, :], func=AF.Identity, scale=wcol(nt), bias=bcol),
             reads=[pst[0], pst[1], V, cbq], writes=[accT])
        for j in range(nt):
            P.op("dve", lambda e, j=j: e.scalar_tensor_tensor(out=accT.t[:], in0=xpT.t[:, j:j + 512], scalar=wcol(j),
                                                              in1=accT.t[:], op0=ALU.mult, op1=ALU.add),
                 reads=[xpT, V, accT], writes=[accT])

    def layer(l, sq):
        mcol = lambda j, k: modT[l].t[:, (j * 8 + k) * NSEQ + sq:(j * 8 + k) * NSEQ + sq + 1]

        def stop(label):
            if dbg and dbg.get("stop") == label and l == dbg.get("nl", DEPTH) - 1:
                pos = rq["next"] % len(order)
                if pos:
                    for s_ in order[pos:]:
                        next_slab(l, s_)
                return True
            return False
        norm(lambda k: hT3[:, k, :], lambda k: hT, lambda k: AT.t[:, (l * 2 + 0) * 8 + k:(l * 2 + 0) * 8 + k + 1],
             lambda k: mcol(0, k), [AT, modT[l]])
        for c in range(16):
            if c % 4 == 0:
                sl = next_slab(l, c // 4)
                sl3 = sl.t[:, 0:4096].rearrange("p (k c) -> p k c", k=8)
            pst = nextps()
            group_mm(pst, lambda pst=pst: pst[0].t[:, :], lambda k, sl3=sl3, c=c: sl3[:, k, (c % 4) * 128:(c % 4 + 1) * 128],
                     lambda k: hT3[:, k, :], 8, [hT, sl])
            cnt["xp"] += 1
            xpT, accT = xp[cnt["xp"] % 2], acc[cnt["xp"] % 2]
            conv_chunk(pst, xpT, accT, 4, tailqk[l], c, f"cw{l}", f"cb{l}", c, 16, vcol(f"inbqk{l}", c), l)
            dstT, dst3 = (bufQ, Q3) if c < 8 else (bufK, K3)
            P.op("act", lambda e, accT=accT, dst3=dst3, c=c: e.activation(out=dst3[:, c % 8, :], in_=accT.t[:], func=AF.Silu),
                 reads=[accT], writes=[dstT])
        sl = next_slab(l, 41)
        slg = sl.t[:, 0:128].rearrange("p (k c) -> p k c", k=8)
        group_mm(psA, lambda: psA[0].t[0:8, :], lambda k, slg=slg: slg[:, k, 0:8], lambda k: hT3[:, k, :], 8, [hT, sl])
        group_mm(psB, lambda: psB[0].t[0:8, :], lambda k, slg=slg: slg[:, k, 8:16], lambda k: hT3[:, k, :], 8, [hT, sl])
        P.op("act", lambda e: e.activation(out=GA.t[0:8, :], in_=psA[0].t[0:8, :], func=AF.Tanh, scale=1.0 / CAP,
                                           bias=gb15.t[0:8, 2 * l:2 * l + 1]), reads=[psA[0], gb15], writes=[GA])
        P.op("act", lambda e: e.activation(out=GF.t[0:8, :], in_=psB[0].t[0:8, :], func=AF.Tanh, scale=1.0 / CAP,
                                           bias=gb15.t[0:8, 2 * l + 1:2 * l + 2]), reads=[psB[0], gb15], writes=[GF])
        P.op("act", lambda e: e.activation(out=GF2.t[0:8, :], in_=GF.t[0:8, :], func=AF.Exp, scale=-CAP), reads=[GF], writes=[GF2])
        P.op("act", lambda e: e.activation(out=GF.t[0:8, :], in_=GF2.t[0:8, :], func=AF.Ln, bias=ones1.t[0:8, 0:1]),
             reads=[GF2, ones1], writes=[GF])
        P.op("dve", lambda e: e.tensor_copy(out=Bcat.t[0:8, 0:1], in_=carry[l].t[0:8, 0:1]), reads=[carry[l]], writes=[Bcat])
        P.op("dve", lambda e: e.tensor_copy(out=ucat.t[0:8, 0:1], in_=carry[l].t[0:8, 1:2]), reads=[carry[l]], writes=[ucat])
        P.op("dve", lambda e: e.tensor_tensor_scan(out=Bcat.t[0:8, 1:513], data0=ones1.t[0:8, 0:1].to_broadcast([8, 512]),
                                                   data1=GF.t[0:8, :], initial=Bcat.t[0:8, 0:1], op0=ALU.mult, op1=ALU.add),
             reads=[GF, Bcat, ones1], writes=[Bcat])
        P.op("dve", lambda e: e.scalar_tensor_tensor(out=GA.t[0:8, :], in0=GA.t[0:8, :], scalar=CAP, in1=Bcat.t[0:8, 1:513],
                                                     op0=ALU.mult, op1=ALU.add), reads=[GA, Bcat], writes=[GA])
        P.op("dve", lambda e: e.tensor_tensor_scan(out=ucat.t[0:8, 1:513], data0=GA.t[0:8, :], data1=GA.t[0:8, :],
                                                   initial=ucat.t[0:8, 0:1], op0=ALU.max, op1=ALU.max),
             reads=[GA, ucat], writes=[ucat])
        P.op("pool", lambda e: e.tensor_copy(out=lhsD.t[32:40, :], in_=GA.t[0:8, :]), reads=[GA], writes=[lhsD])
        P.op("dve", lambda e: e.tensor_tensor(out=r3(ARG.t[0:8, :], 8), in0=bcast_mid(ucat, 0, 8, 0, 8, 64, 64),
                                              in1=r3(ucat.t[0:8, 1:513], 8), op=ALU.subtract), reads=[ucat], writes=[ARG])
        P.op("dve", lambda e: e.tensor_tensor(out=ARG.t[32:40, :], in0=Bcat.t[0:8, 1:513], in1=ucat.t[0:8, 1:513],
                                              op=ALU.subtract), reads=[ucat, Bcat], writes=[ARG])
        P.op("dve", lambda e: e.scalar_tensor_tensor(out=r3(ARG.t[64:72, :], 8), in0=r3(GA.t[0:8, :], 8), scalar=LNSCALE,
                                                     in1=bcast_mid(ucat, 0, 8, 64, 8, 64, 64), op0=ALU.add, op1=ALU.subtract),
             reads=[GA, ucat], writes=[ARG])
        P.op("act", lambda e: e.activation(out=GE.t[0:72, :], in_=ARG.t[0:72, :], func=AF.Exp), reads=[ARG], writes=[GE])
        P.op("dve", lambda e: e.tensor_copy(out=carry[l].t[0:8, 0:1], in_=Bcat.t[0:8, 512:513]), reads=[Bcat], writes=[carry[l]])
        P.op("dve", lambda e: e.tensor_copy(out=carry[l].t[0:8, 1:2], in_=ucat.t[0:8, 512:513]), reads=[ucat], writes=[carry[l]])

        def gtr(e):
            r = None
            for j in range(4):
                r = e.transpose(psA[0].t[:, j * 72:(j + 1) * 72], GE.t[0:72, j * 128:(j + 1) * 128], identF.t[0:72, 0:72])
            return r
        P.op("pe", gtr, reads=[GE, identF], writes=[psA[0], psA[1]])
        P.op("dve", lambda e: e.tensor_copy(out=GT.t[:, :], in_=psA[0].t[:, 0:288]), reads=[psA[0], psA[1]], writes=[GT])
        GT3 = r3(GT.t[:], 4)
        P.dma("sp", "nw", lambda e: [e.dma_start(out=nw.t[:, :], in_=nrm[l, 0:1, :].partition_broadcast(128))], writes=[nw])
        for i in range(2):
            tm_slab(l, 8 + i, lambda j, pst, i=i: P.op(
                "act", lambda e: e.activation(out=vt3[:, j, i * 512:(i + 1) * 512], in_=pst[0].t[:, :], func=AF.Identity),
                reads=[pst[0], pst[1]], writes=[vtok]))
        for i in range(2):
            tm_slab(l, 10 + i, lambda j, pst, i=i: P.op(
                "act", lambda e: e.activation(out=Gp3[:, j, i * 512:(i + 1) * 512], in_=pst[0].t[:, :], func=AF.Sigmoid),
                reads=[pst[0], pst[1]], writes=[Gp]))

        def gate2(j, pst, i, func):
            cnt["tg"] += 1
            tgt = tg[cnt["tg"] % 2]
            P.op("act", lambda e: e.activation(out=tgt.t[:], in_=pst[0].t[:, :], func=func),
                 reads=[pst[0], pst[1]], writes=[tgt])
            P.op("dve", lambda e: e.tensor_tensor(out=Gp3[:, j, i * 512:(i + 1) * 512], in0=Gp3[:, j, i * 512:(i + 1) * 512],
                                                  in1=tgt.t[:], op=ALU.mult), reads=[Gp, tgt], writes=[Gp])
            P.op("dve", lambda e: e.tensor_tensor(out=Gp3[:, j, i * 512:(i + 1) * 512], in0=Gp3[:, j, i * 512:(i + 1) * 512],
                                                  in1=nw.t[:, i * 512:(i + 1) * 512], op=ALU.mult), reads=[Gp, nw], writes=[Gp])
        for i in range(2):
            tm_slab(l, 12 + i, lambda j, pst, i=i: gate2(j, pst, i, AF.Sigmoid))

        CB3 = r3(CextB[l].t[:], 8)
        C3 = r3(Cext[l].t[:], 8)
        msegs = []
        for c in range(8):
            hb, j = c % 2, c // 2
            hs = HS(hb)
            cnt["c"] += 1
            sl_ = cnt["c"] % 2
            rD, wb, qs_, dc, kh = rhsD[sl_], wibd[sl_], qsc[sl_], dec[sl_], khat[sl_]
            c0 = c * 64
            sg = {"A": [], "B": [], "C": []}
            msegs.append(sg)
            P.capture = sg["A"]
            P.op("dve", lambda e, c0=c0, rD=rD: e.tensor_tensor(
                out=r3(rD.t[0:8, :], 8), in0=bass.AP(ucat.t, ucat.t[0:8, 1 + c0:2 + c0].offset,
                                                     [[ucat.t[0:8, 0:1].ap[0][0], 8], [0, 8], [1, 64]]),
                in1=r3(negbm8.t[0:8, :], 8), op=ALU.mult), reads=[ucat, negbm8], writes=[rD])
            P.op("dve", lambda e, c0=c0, wb=wb: e.tensor_tensor(
                out=r3(wb.t[0:8, :], 8), in0=bass.AP(GE.t, GE.t[0:8, c0:c0 + 1].offset,
                                                     [[GE.t[0:8, 0:1].ap[0][0], 8], [0, 8], [1, 64]]),
                in1=r3(negbm8.t[0:8, :], 8), op=ALU.mult), reads=[GE, negbm8], writes=[wb])

            def smm(e, c0=c0, hs=hs, hb=hb):
                r = None
                for h in range(8):
                    r = e.matmul(psS[hb].t[hs, h * 64:(h + 1) * 64], lhsT=K3[:, h, c0:c0 + 64], rhs=Q3[:, h, c0:c0 + 64],
                                 start=True, stop=True, skip_group_check=True)
                return r
            P.op("pe", smm, reads=[bufQ, bufK], writes=[psS[hb]])
            P.op("pe", lambda e, c0=c0, hs=hs, hb=hb, rD=rD: e.matmul(psD[hb].t[hs, :], lhsT=lhsD.t[:, c0:c0 + 64], rhs=rD.t[:, :],
                                                               start=True, stop=True), reads=[lhsD, rD], writes=[psD[hb]])
            P.op("pe", lambda e, wb=wb: e.matmul(psWI[0].t[:, :], lhsT=negonesF.t[0:8, :], rhs=wb.t[0:8, :], start=True, stop=True),
                 reads=[negonesF, wb], writes=[psWI[0], psWI[1]])
            P.op("act", lambda e, hs=hs, hb=hb: e.activation(out=DT[hb].t[hs, :], in_=psD[hb].t[hs, :], func=AF.Exp),
                 reads=[psD[hb]], writes=[DT[hb]])
            P.op("dve", lambda e, hs=hs, hb=hb: e.tensor_tensor(out=wT[hb].t[hs, :], in0=psS[hb].t[hs, :], in1=DT[hb].t[hs, :],
                                                                op=ALU.mult), reads=[psS[hb], DT[hb]], writes=[wT[hb]])
            P.op("dve", lambda e, c0=c0, qs_=qs_: e.tensor_tensor(out=r3(qs_.t[:], 8), in0=Q3[:, :, c0:c0 + 64],
                                                                  in1=r3(psWI[0].t[:, :], 8), op=ALU.mult),
                 reads=[bufQ, psWI[0], psWI[1]], writes=[qs_])
            P.op("dve", lambda e, dc=dc: e.tensor_copy(out=dc.t[:, :], in_=r3(psWI[0].t[:, :], 8)[:, :, 63]),
                 reads=[psWI[0], psWI[1]], writes=[dc])
            qs3 = r3(qs_.t[:], 8)

            def hmm(e, hs=hs, hb=hb, j=j, qs3=qs3):
                r = None
                for h in range(8):
                    pH = (psH0, psH1)[h // 4][hb]
                    o_ = pH.t[hs, (h % 4) * 128:(h % 4 + 1) * 128]
                    e.matmul(o_, lhsT=wT[hb].t[hs, h * 64:(h + 1) * 64], rhs=vt3[hs, j, h * 128:(h + 1) * 128],
                             start=(h % 4 == 0), stop=False, skip_group_check=True)
                    r = e.matmul(o_, lhsT=qs3[:, h, :], rhs=CB3[:, h, :], start=False, stop=True, skip_group_check=True)
                for h in range(8):
                    o_ = psX[hb].t[hs, 256 + h:257 + h]
                    e.matmul(o_, lhsT=wT[hb].t[hs, h * 64:(h + 1) * 64], rhs=onesB.t[hs, 0:1],
                             start=(h == 0), stop=False, skip_group_check=True)
                    r = e.matmul(o_, lhsT=qs3[:, h, :], rhs=nB[l].t[:, h:h + 1], start=False, stop=True, skip_group_check=True)
                return r
            P.capture = sg["B"]
            P.op("pe", hmm, reads=[wT[hb], vtok, qs_, CextB[l], nB[l], onesB], writes=[psH0[hb], psH1[hb], psX[hb]])
            S_ = sm[hb].t
            P.op("act", lambda e, hs=hs, hb=hb: e.activation(out=E01t[hs, 0:512], in_=psH0[hb].t[hs, :], func=AF.Identity),
                 reads=[psH0[hb]], writes=[E01[hb]])
            P.op("act", lambda e, hs=hs, hb=hb: e.activation(out=E01t[hs, 512:1024], in_=psH1[hb].t[hs, :], func=AF.Identity),
                 reads=[psH1[hb]], writes=[E01[hb]])
            P.op("dve", lambda e, hs=hs, hb=hb, S_=S_: e.tensor_copy(out=S_[hs, 56:64], in_=psX[hb].t[hs, 256:264]),
                 reads=[psX[hb]], writes=[sm[hb]])
            P.capture = sg["C"]
            P.op("dve", lambda e, hs=hs, hb=hb, j=j, S_=S_: e.tensor_tensor(out=S_[hs, 48:56], in0=S_[hs, 56:64],
                                                                        in1=GT3[hs, j, 32:40], op=ALU.max),
                 reads=[sm[hb], GT], writes=[sm[hb]])
            P.op("dve", lambda e, hs=hs, hb=hb, S_=S_: e.scalar_tensor_tensor(out=S_[hs, 0:8], in0=S_[hs, 56:64], scalar=-1.0,
                                                                          in1=S_[hs, 48:56], op0=ALU.mult, op1=ALU.max),
                 reads=[sm[hb]], writes=[sm[hb]])
            P.op("dve", lambda e, hs=hs, S_=S_: e.reciprocal(out=S_[hs, 8:16], in_=S_[hs, 0:8]), reads=[sm[hb]], writes=[sm[hb]])
            P.op("act", lambda e, hs=hs, hb=hb: e.activation(out=sqht[hs, :], in_=E01t[hs, :], func=AF.Square),
                 reads=[E01[hb]], writes=[sqh[hb]])
            P.op("dve", lambda e, hs=hs, S_=S_: e.tensor_reduce(out=S_[hs, 16:24], in_=r3(sqht[hs, :], 8), axis=AX.X, op=ALU.add),
                 reads=[sqh[hb]], writes=[sm[hb]])
            P.op("dve", lambda e, hs=hs, S_=S_: e.tensor_tensor(out=S_[hs, 24:32], in0=S_[hs, 8:16], in1=S_[hs, 8:16], op=ALU.mult),
                 reads=[sm[hb]], writes=[sm[hb]])
            P.op("dve", lambda e, hs=hs, S_=S_: e.tensor_tensor(out=S_[hs, 24:32], in0=S_[hs, 24:32], in1=S_[hs, 16:24], op=ALU.mult),
                 reads=[sm[hb]], writes=[sm[hb]])
            P.op("dve", lambda e, hs=hs, S_=S_: e.tensor_scalar(out=S_[hs, 24:32], in0=S_[hs, 24:32], scalar1=1.0 / 128, scalar2=EPS,
                                                                op0=ALU.mult, op1=ALU.add), reads=[sm[hb]], writes=[sm[hb]])
            P.op("act", lambda e, hs=hs, S_=S_: e.activation(out=S_[hs, 32:40], in_=S_[hs, 24:32], func=AF.Ln),
                 reads=[sm[hb]], writes=[sm[hb]])
            P.op("act", lambda e, hs=hs, S_=S_: e.activation(out=S_[hs, 32:40], in_=S_[hs, 32:40], func=AF.Exp, scale=-0.5),
                 reads=[sm[hb]], writes=[sm[hb]])
            P.op("dve", lambda e, hs=hs, S_=S_: e.tensor_tensor(out=S_[hs, 40:48], in0=S_[hs, 32:40], in1=S_[hs, 8:16], op=ALU.mult),
                 reads=[sm[hb]], writes=[sm[hb]])
            P.op("pool", lambda e, hs=hs, S_=S_: e.tensor_tensor(
                out=r3(E01t[hs, :], 8), in0=r3(E01t[hs, :], 8),
                in1=bass.AP(smt, S_[hs, 40:41].offset, [[S_[hs, 0:1].ap[0][0], 64], [1, 8], [0, 128]]),
                op=ALU.mult), reads=[sm[hb]], writes=[E01[hb]])
            P.op("pool", lambda e, hs=hs, j=j: e.tensor_tensor(
                out=ya3[hs, j, :], in0=E01t[hs, :], in1=Gp3[hs, j, :], op=ALU.mult), reads=[E01[hb], Gp], writes=[yacc])
            P.capture = sg["A"]
            psXb = psX[hb].t[:, :].bitcast(BF16)
            kh3 = r3(kh[hb].t[hs, :], 8)
            for q in range(2):
                def ktr(e, q=q, c0=c0, hs=hs, psXb=psXb):
                    r = None
                    for hh in range(4):
                        h = 4 * q + hh
                        r = e.transpose(psXb[hs, hh * 128:(hh + 1) * 128], K3[:, h, c0:c0 + 64], identB.t[:, :])
                    return r
                P.op("pe", ktr, reads=[bufK, identB], writes=[psX[hb]])
                P.op("dve", lambda e, q=q, hs=hs, j=j, psXb=psXb, kh3=kh3: e.tensor_tensor(
                    out=kh3[:, 4 * q:4 * q + 4, :], in0=r3(psXb[hs, 0:512], 4),
                    in1=bass.AP(GT.t, GT3[hs, j, 64 + 4 * q:65 + 4 * q].offset, [[GT.t[hs, 0:1].ap[0][0], 64], [1, 4], [0, 128]]),
                    op=ALU.mult), reads=[psX[hb], GT], writes=[kh[hb]])

            def cmm(e, hs=hs, j=j, kh3=kh3):
                r = None
                for h in range(8):
                    pc = psAB[h // 4][0]
                    r = e.matmul(pc.t[:, (h % 4) * 128:(h % 4 + 1) * 128], lhsT=kh3[:, h, :], rhs=vt3[hs, j, h * 128:(h + 1) * 128],
                                 start=(h % 4 == 0), stop=True, skip_group_check=True)
                for h in range(8):
                    r = e.matmul(psWI[0].t[:, h:h + 1], lhsT=kh3[:, h, :], rhs=onesB.t[hs, 0:1],
                                 start=(h == 0), stop=True, skip_group_check=True)
                return r
            P.capture = sg["B"]
            P.op("pe", cmm, reads=[kh[hb], vtok, onesB], writes=[psA[0], psA[1], psB[0], psB[1], psWI[0], psWI[1]])
            for h in range(8):
                pc = psAB[h // 4]
                P.op("dve", lambda e, h=h, pc=pc, dc=dc: e.scalar_tensor_tensor(
                    out=CB3[:, h, :], in0=C3[:, h, :], scalar=dc.t[:, h:h + 1], in1=pc[0].t[:, (h % 4) * 128:(h % 4 + 1) * 128],
                    op0=ALU.mult, op1=ALU.add), reads=[Cext[l], dc, pc[0], pc[1]], writes=[CextB[l]])
            P.op("dve", lambda e, dc=dc: e.tensor_tensor(out=nF[l].t[:, :], in0=nF[l].t[:, :], in1=dc.t[:, :], op=ALU.mult),
                 reads=[nF[l], dc], writes=[nF[l]])
            P.op("dve", lambda e: e.tensor_tensor(out=nF[l].t[:, :], in0=nF[l].t[:, :], in1=psWI[0].t[:, 0:8], op=ALU.add),
                 reads=[nF[l], psWI[0], psWI[1]], writes=[nF[l]])
            P.op("dve", lambda e: e.tensor_copy(out=nB[l].t[:, :], in_=nF[l].t[:, :]), reads=[nF[l]], writes=[nB[l]])
            for h in range(8):
                pc = psAB[h // 4]
                P.op("dve", lambda e, h=h, pc=pc, dc=dc: e.scalar_tensor_tensor(
                    out=C3[:, h, :], in0=C3[:, h, :], scalar=dc.t[:, h:h + 1], in1=pc[0].t[:, (h % 4) * 128:(h % 4 + 1) * 128],
                    op0=ALU.mult, op1=ALU.add), reads=[Cext[l], dc, pc[0], pc[1]], writes=[Cext[l]])
            P.capture = None
        P.pipeline(msegs)

        if dbg and dbg.get("stage") == "mlstm":
            for s_ in order[order.index(13) + 1:]:
                next_slab(l, s_)
            return
        slq = {}
        for which in range(2):
            for h in range(8):
                key = (which, h // 4)
                if key not in slq:
                    slq[key] = next_slab(l, (4 if which == 0 else 6) + h // 4)
                sl = slq[key]
                sl3 = sl.t[:, 0:4096].rearrange("p (k c) -> p k c", k=8)
                pst = nextps()
                group_mm(pst, lambda pst=pst: pst[0].t[:, :], lambda k, sl3=sl3, h=h: sl3[:, k, (h % 4) * 128:(h % 4 + 1) * 128],
                         lambda k: hT3[:, k, :], 8, [hT, sl])
                if which == 0:
                    P.op("act", lambda e, pst=pst, h=h: e.activation(out=Q3[:, h, :], in_=pst[0].t[:, :], func=AF.Silu,
                                                                    bias=vcol(f"inbhq{l}", h)), reads=[pst[0], pst[1], V], writes=[bufQ])
                else:
                    sg_, lf_, G_ = tmpF[0], tmpF[1], tmpF[2]
                    P.op("act", lambda e, pst=pst, h=h: e.activation(out=sg_.t[:], in_=pst[0].t[:, :], func=AF.Sigmoid,
                                                                    bias=vcol(f"inbhf{l}", h)), reads=[pst[0], pst[1], V], writes=[sg_])
                    P.op("act", lambda e, h=h: e.activation(out=lf_.t[:], in_=sg_.t[:], func=AF.Ln,
                                                           scale=OML.t[:, l * 8 + h:l * 8 + h + 1], bias=LB.t[:, l * 8 + h:l * 8 + h + 1]),
                         reads=[sg_, OML, LB], writes=[lf_])
                    P.op("dve", lambda e, h=h: e.tensor_scalar(out=kkb.t[:], in0=sg_.t[:], scalar1=NOML.t[:, l * 8 + h:l * 8 + h + 1],
                                                              scalar2=OML.t[:, l * 8 + h:l * 8 + h + 1], op0=ALU.mult, op1=ALU.add),
                         reads=[sg_, NOML, OML], writes=[kkb])
                    P.op("dve", lambda e: e.tensor_tensor_scan(out=G_.t[:], data0=ones1.t[:, 0:1].to_broadcast([128, 512]),
                                                               data1=lf_.t[:], initial=0.0, op0=ALU.mult, op1=ALU.add),
                         reads=[lf_, ones1], writes=[G_])
                    P.op("dve", lambda e: e.tensor_tensor(out=r3(lf_.t[:], 16), in0=r3(G_.t[:], 16),
                                                          in1=bcast_mid(G_, 0, 128, 15, 16, 32, 32), op=ALU.subtract),
                         reads=[G_], writes=[lf_])
                    FA3 = FA.t[:, :].rearrange("p (h n) -> p h n", h=8)
                    gstep = G_.t[:, 0:1].ap[0][0]
                    P.op("dve", lambda e, h=h: e.tensor_tensor(
                        out=FA3[:, h, 0:15], in0=bass.AP(G_.t, 47, [[gstep, 128], [32, 15]]),
                        in1=bass.AP(G_.t, 15, [[gstep, 128], [32, 15]]), op=ALU.subtract), reads=[G_], writes=[FA])
                    P.op("dve", lambda e, h=h: e.tensor_tensor(out=FA3[:, h, 15:16], in0=G_.t[:, 511:512], in1=G_.t[:, 495:496],
                                                              op=ALU.subtract), reads=[G_], writes=[FA])
                    P.op("dve", lambda e, h=h: e.tensor_copy(out=FA3[:, h, 16:17], in_=G_.t[:, 15:16]), reads=[G_], writes=[FA])
                    P.op("act", lambda e: e.activation(out=epb.t[:], in_=lf_.t[:], func=AF.Exp), reads=[lf_], writes=[epb])
                    P.op("act", lambda e: e.activation(out=enb.t[:], in_=lf_.t[:], func=AF.Exp, scale=-1.0), reads=[lf_], writes=[enb])
                    P.op("dve", lambda e, h=h: e.tensor_tensor(out=Q3[:, h, :], in0=Q3[:, h, :], in1=epb.t[:], op=ALU.mult),
                         reads=[bufQ, epb], writes=[bufQ])
                    P.op("dve", lambda e, h=h: e.tensor_tensor(out=K3[:, h, :], in0=kkb.t[:], in1=enb.t[:], op=ALU.mult),
                         reads=[kkb, enb], writes=[bufK])
        if stop("hg_in"):
            return
        P.op("act", lambda e: e.activation(out=FX.t[:, :], in_=FA.t[:, :], func=AF.Exp), reads=[FA], writes=[FX])
        FX3 = FX.t[:, :].rearrange("p (h n) -> p h n", h=8)
        P.dma("sp", "nw", lambda e: [e.dma_start(out=nw.t[:, :], in_=nrm[l, 1:2, :].partition_broadcast(128))], writes=[nw])
        for i in range(2):
            tm_slab(l, 14 + i, lambda j, pst, i=i: P.op(
                "act", lambda e: e.activation(out=vt3[:, j, i * 512:(i + 1) * 512], in_=pst[0].t[:, :], func=AF.Identity),
                reads=[pst[0], pst[1]], writes=[vtok]))
        for i in range(2):
            tm_slab(l, 16 + i, lambda j, pst, i=i: P.op(
                "act", lambda e: e.activation(out=Gp3[:, j, i * 512:(i + 1) * 512], in_=pst[0].t[:, :], func=AF.Silu),
                reads=[pst[0], pst[1]], writes=[Gp]))
        for i in range(2):
            tm_slab(l, 18 + i, lambda j, pst, i=i: gate2(j, pst, i, AF.Sigmoid))
        T3 = r3(Tst[l].t[:], 8)
        TB3 = r3(TstB[l].t[:], 8)
        fstep = FX.t[:, 0:1].ap[0][0]
        P.op("dve", lambda e: e.tensor_tensor(out=T3, in0=T3, in1=bass.AP(FX.t, 16, [[fstep, 128], [17, 8], [0, 128]]), op=ALU.mult),
             reads=[Tst[l], FX], writes=[Tst[l]])
        P.op("act", lambda e: e.activation(out=TstB[l].t[:, :], in_=Tst[l].t[:, :], func=AF.Identity), reads=[Tst[l]], writes=[TstB[l]])
        if stop("hg_tm"):
            return
        hsegs = []
        for pr in range(8):
            hb, j = pr % 2, pr // 2
            hs = HS(hb)
            cnt["c"] += 1
            kh0 = khat[0]
            kh1 = khat[1]
            qp = Qpad[cnt["c"] % 2]
            c0 = pr * 64
            sg = {"A": [], "B": [], "C": []}
            hsegs.append(sg)
            P.capture = sg["A"]

            def smm2(e, c0=c0, hs=hs, hb=hb):
                r = None
                for h in range(8):
                    r = e.matmul(psS[hb].t[hs, h * 64:(h + 1) * 64], lhsT=K3[:, h, c0:c0 + 64], rhs=Q3[:, h, c0:c0 + 64],
                                 start=True, stop=True, skip_group_check=True)
                return r
            P.op("pe", smm2, reads=[bufQ, bufK], writes=[psS[hb]])
            P.op("dve", lambda e, hs=hs, hb=hb: e.tensor_tensor(out=wT[hb].t[hs, :], in0=psS[hb].t[hs, :], in1=mask01.t[hs, :],
                                                                op=ALU.mult), reads=[psS[hb], mask01], writes=[wT[hb]])
            P.op("pool", lambda e, qp=qp, c0=c0: e.tensor_copy(out=r3(qp.t[:], 8)[:, :, 32:64], in_=Q3[:, :, c0 + 32:c0 + 64]),
                 reads=[bufQ], writes=[qp])
            psXb = psX[hb].t[:, :].bitcast(BF16)
            k03 = r3(kh0[hb].t[hs, :], 8)
            k13 = r3(kh1[hb].t[hs, :], 8)
            for q in range(2):
                def ktr2(e, q=q, c0=c0, hs=hs, psXb=psXb):
                    r = None
                    for hh in range(4):
                        h = 4 * q + hh
                        r = e.transpose(psXb[hs, hh * 128:(hh + 1) * 128], K3[:, h, c0:c0 + 64], identB.t[:, :])
                    return r
                P.op("pe", ktr2, reads=[bufK, identB], writes=[psX[hb]])
                P.op("act", lambda e, q=q, hs=hs, psXb=psXb, k03=k03: e.activation(
                    out=k03[:, 4 * q:4 * q + 4, :], in_=r3(psXb[hs, 0:512], 4), func=AF.Identity, scale=rm.t[hs, 0:1]),
                    reads=[psX[hb], rm], writes=[kh0[hb]])
                P.op("act", lambda e, q=q, hs=hs, psXb=psXb, k13=k13: e.activation(
                    out=k13[:, 4 * q:4 * q + 4, :], in_=r3(psXb[hs, 0:512], 4), func=AF.Identity, scale=rm.t[hs, 1:2]),
                    reads=[psX[hb], rm], writes=[kh1[hb]])

            def omm(e, hs=hs, hb=hb, j=j, c0=c0):
                r = None
                for h in range(8):
                    pH = (psH0, psH1)[h // 4][hb]
                    o_ = pH.t[hs, (h % 4) * 128:(h % 4 + 1) * 128]
                    e.matmul(o_, lhsT=wT[hb].t[hs, h * 64:(h + 1) * 64], rhs=vt3[hs, j, h * 128:(h + 1) * 128],
                             start=(h % 4 == 0), stop=False, skip_group_check=True)
                    o0 = pH.t[64 * hb:64 * hb + 32, (h % 4) * 128:(h % 4 + 1) * 128]
                    r = e.matmul(o0, lhsT=Q3[:, h, c0:c0 + 32], rhs=TB3[:, h, :], start=False, stop=False, skip_group_check=True)
                return r
            P.capture = sg["B"]
            P.op("pe", omm, reads=[wT[hb], vtok, bufQ, TstB[l]], writes=[psH0[hb], psH1[hb]])
            for sub in range(2):
                cc = 2 * pr + sub
                k3 = (k03, k13)[sub]
                khT = (kh0, kh1)[sub][hb]

                def umm(e, hs=hs, j=j, k3=k3):
                    r = None
                    for h in range(8):
                        pc = psAB[h // 4][0]
                        r = e.matmul(pc.t[:, (h % 4) * 128:(h % 4 + 1) * 128], lhsT=k3[:, h, :], rhs=vt3[hs, j, h * 128:(h + 1) * 128],
                                     start=(h % 4 == 0), stop=True, skip_group_check=True)
                    return r
                P.op("pe", umm, reads=[khT, vtok], writes=[psA[0], psA[1], psB[0], psB[1]])
                for q in range(2):
                    pc = psAB[q]
                    P.op("dve", lambda e, q=q, pc=pc: e.tensor_tensor(out=T3[:, 4 * q:4 * q + 4, :], in0=T3[:, 4 * q:4 * q + 4, :],
                                                                      in1=r3(pc[0].t[:, :], 4), op=ALU.add),
                         reads=[Tst[l], pc[0], pc[1]], writes=[Tst[l]])
                for q in range(2):
                    P.op("dve", lambda e, q=q, cc=cc: e.tensor_tensor(
                        out=TB3[:, 4 * q:4 * q + 4, :], in0=T3[:, 4 * q:4 * q + 4, :],
                        in1=bass.AP(FX.t, 68 * q + cc, [[fstep, 128], [17, 4], [0, 128]]), op=ALU.mult),
                        reads=[Tst[l], FX], writes=[TstB[l]])
                for q in range(2):
                    P.op("dve", lambda e, q=q, cc=cc: e.tensor_tensor(
                        out=T3[:, 4 * q:4 * q + 4, :], in0=T3[:, 4 * q:4 * q + 4, :],
                        in1=bass.AP(FX.t, 68 * q + cc, [[fstep, 128], [17, 4], [0, 128]]), op=ALU.mult),
                        reads=[Tst[l], FX], writes=[Tst[l]])
                if sub == 0:
                    qp3 = r3(qp.t[:], 8)

                    def omm2(e, hs=hs, hb=hb, qp3=qp3):
                        r = None
                        for h in range(8):
                            pH = (psH0, psH1)[h // 4][hb]
                            o_ = pH.t[hs, (h % 4) * 128:(h % 4 + 1) * 128]
                            r = e.matmul(o_, lhsT=qp3[:, h, :], rhs=TB3[:, h, :], start=False, stop=True, skip_group_check=True)
                        return r
                    P.op("pe", omm2, reads=[qp, TstB[l]], writes=[psH0[hb], psH1[hb]])
            S_ = sm[hb].t
            P.op("act", lambda e, hs=hs, hb=hb: e.activation(out=E01t[hs, 0:512], in_=psH0[hb].t[hs, :], func=AF.Identity),
                 reads=[psH0[hb]], writes=[E01[hb]])
            P.op("act", lambda e, hs=hs, hb=hb: e.activation(out=E01t[hs, 512:1024], in_=psH1[hb].t[hs, :], func=AF.Identity),
                 reads=[psH1[hb]], writes=[E01[hb]])
            P.capture = sg["C"]
            P.op("act", lambda e, hs=hs, hb=hb: e.activation(out=sqht[hs, :], in_=E01t[hs, :], func=AF.Square),
                 reads=[E01[hb]], writes=[sqh[hb]])
            P.op("dve", lambda e, hs=hs, S_=S_: e.tensor_reduce(out=S_[hs, 16:24], in_=r3(sqht[hs, :], 8), axis=AX.X, op=ALU.add),
                 reads=[sqh[hb]], writes=[sm[hb]])
            P.op("dve", lambda e, hs=hs, S_=S_: e.tensor_scalar(out=S_[hs, 24:32], in0=S_[hs, 16:24], scalar1=1.0 / 128, scalar2=EPS,
                                                                op0=ALU.mult, op1=ALU.add), reads=[sm[hb]], writes=[sm[hb]])
            P.op("act", lambda e, hs=hs, S_=S_: e.activation(out=S_[hs, 40:48], in_=S_[hs, 24:32], func=AF.Ln),
                 reads=[sm[hb]], writes=[sm[hb]])
            P.op("act", lambda e, hs=hs, S_=S_: e.activation(out=S_[hs, 40:48], in_=S_[hs, 40:48], func=AF.Exp, scale=-0.5),
                 reads=[sm[hb]], writes=[sm[hb]])
            P.op("pool", lambda e, hs=hs, S_=S_: e.tensor_tensor(
                out=r3(E01t[hs, :], 8), in0=r3(E01t[hs, :], 8),
                in1=bass.AP(smt, S_[hs, 40:41].offset, [[S_[hs, 0:1].ap[0][0], 64], [1, 8], [0, 128]]),
                op=ALU.mult), reads=[sm[hb]], writes=[E01[hb]])
            for q in range(2):
                cnt["tg"] += 1
                tgt = tg[cnt["tg"] % 2]
                P.op("pool", lambda e, hs=hs, q=q, j=j, tgt=tgt: e.tensor_tensor(
                    out=tgt.t[hs, :], in0=E01t[hs, q * 512:(q + 1) * 512],
                    in1=Gp3[hs, j, q * 512:(q + 1) * 512], op=ALU.mult), reads=[E01[hb], Gp], writes=[tgt])
                P.op("pool", lambda e, hs=hs, q=q, j=j, tgt=tgt: e.tensor_tensor(
                    out=ya3[hs, j, q * 512:(q + 1) * 512], in0=ya3[hs, j, q * 512:(q + 1) * 512], in1=tgt.t[hs, :], op=ALU.add),
                    reads=[yacc, tgt], writes=[yacc])
            P.capture = None
        P.pipeline(hsegs)

        if stop("hg_loop"):
            return
        for j in range(4):
            psXb = psX[0].t[:, :].bitcast(BF16)

            def ytr(e, j=j, psXb=psXb):
                r = None
                for f in range(8):
                    r = e.transpose(psXb[:, f * 128:(f + 1) * 128], ya3[:, j, f * 128:(f + 1) * 128], identB.t[:, :])
                return r
            P.op("pe", ytr, reads=[yacc, identB], writes=[psX[0], psX[1]])
            P.op("act", lambda e, j=j, psXb=psXb: e.activation(out=yT3[:, :, j * 128:(j + 1) * 128], in_=r3(psXb[:, :], 8),
                                                              func=AF.Identity), reads=[psX[0], psX[1]], writes=[yT])
        if stop("ytr"):
            return
        for jo in range(8):
            if jo % 4 == 0:
                sl = next_slab(l, 20 + jo // 4)
                sl3 = sl.t[:, 0:4096].rearrange("p (k c) -> p k c", k=8)
            pst = nextps()
            group_mm(pst, lambda pst=pst: pst[0].t[:, :], lambda k, sl3=sl3, jo=jo: sl3[:, k, (jo % 4) * 128:(jo % 4 + 1) * 128],
                     lambda k: yT3[:, k, :], 8, [yT, sl])
            P.op("dve", lambda e, pst=pst, jo=jo: e.scalar_tensor_tensor(out=xall[:, jo, :], in0=pst[0].t[:, :], scalar=mcol(2, jo),
                                                                         in1=xall[:, jo, :], op0=ALU.mult, op1=ALU.add),
                 reads=[pst[0], pst[1], modT[l], xTc[jo]], writes=[xTc[jo]])
        if dbg and dbg.get("stage") == "mix" and l == dbg.get("nl", DEPTH) - 1:
            for s_ in list(range(22, 33)) + list(range(33, 41)):
                next_slab(l, s_)
            return
        norm(lambda k: hT3[:, k, :], lambda k: hT, lambda k: AT.t[:, (l * 2 + 1) * 8 + k:(l * 2 + 1) * 8 + k + 1],
             lambda k: mcol(3, k), [AT, modT[l]])

        def gchunk(jj):
            if jj < 8:
                return bufQ, Q3[:, jj, :]
            if jj < 16:
                return bufK, K3[:, jj - 8, :]
            return vtok, vtok.t[:, (jj - 16) * 512:(jj - 16 + 1) * 512]
        for s in range(11):
            sl = next_slab(l, 22 + s)
            sl3 = sl.t[:, 0:4096].rearrange("p (k c) -> p k c", k=8)
            for u in range(2):
                jj = 2 * s + u
                res = {}
                for part in range(2):
                    col = part * 256 + u * 128
                    pst = nextps()
                    group_mm(pst, lambda pst=pst: pst[0].t[:, :], lambda k, sl3=sl3, col=col: sl3[:, k, col:col + 128],
                             lambda k: hT3[:, k, :], 8, [hT, sl])
                    cnt["xp"] += 1
                    xpT, accT = xp[cnt["xp"] % 2], acc[cnt["xp"] % 2]
                    cidx = jj + 22 * part
                    conv_chunk(pst, xpT, accT, 3, tailff[l], cidx, f"fcw{l}", f"fcb{l}", cidx, 44, None, l)
                    res[part] = accT
                cnt["tg"] += 1
                sgt = tg[cnt["tg"] % 2]
                P.op("act", lambda e, sgt=sgt, a=res[0]: e.activation(out=sgt.t[:], in_=a.t[:], func=AF.Silu), reads=[res[0]], writes=[sgt])
                gT_, gap = gchunk(jj)
                P.op("dve", lambda e, sgt=sgt, a=res[1], gap=gap: e.tensor_tensor(out=gap, in0=sgt.t[:], in1=a.t[:], op=ALU.mult),
                     reads=[sgt, res[1]], writes=[gT_])
        for jo in range(8):
            sl = next_slab(l, 33 + jo)
            pst = nextps()
            group_mm(pst, lambda pst=pst: pst[0].t[:, :], lambda k, sl=sl: sl.t[:, k * 128:(k + 1) * 128],
                     lambda k: gchunk(k)[1], FFC, [bufQ, bufK, vtok, sl])
            P.op("dve", lambda e, pst=pst, jo=jo: e.scalar_tensor_tensor(out=xall[:, jo, :], in0=pst[0].t[:, :], scalar=mcol(5, jo),
                                                                         in1=xall[:, jo, :], op0=ALU.mult, op1=ALU.add),
                 reads=[pst[0], pst[1], modT[l], xTc[jo]], writes=[xTc[jo]])

    for sq in range(NSEQ):
        for l in range(DEPTH):
            for tl_ in (Cext[l], CextB[l], nF[l], nB[l], Tst[l], TstB[l], tailqk[l], tailff[l], carry[l]):
                P.op("pool", lambda e, tl_=tl_: e.memset(tl_.t[:, :], 0.0), writes=[tl_])
            for w_ in range(2):
                sc_ = lambda k: modT[l].t[:, ((1 + 3 * w_) * 8) * NSEQ + sq: ((1 + 3 * w_) * 8 + 8) * NSEQ + sq]
                P.op("dve", lambda e, l=l, w_=w_, sq=sq: e.scalar_tensor_tensor(
                    out=AT.t[:, (l * 2 + w_) * 8:(l * 2 + w_) * 8 + 8],
                    in0=bass.AP(modT[l].t, modT[l].t[:, ((1 + 3 * w_) * 8) * NSEQ + sq:((1 + 3 * w_) * 8) * NSEQ + sq + 1].offset,
                                [[modT[l].t[:, 0:1].ap[0][0], 128], [NSEQ, 8]]),
                    scalar=1.0, in1=vcol(f"mixw{l}" if w_ == 0 else f"ffnw{l}", 0, 8), op0=ALU.add, op1=ALU.mult),
                    reads=[modT[l], V], writes=[AT])
        for ti in range(NT):
            P.dma("sp", "xin", lambda e, sq=sq, ti=ti: [e.dma_start(out=xall, in_=xin[sq, :, :, ti * TT:(ti + 1) * TT])],
                  writes=xTc)
            for l in range(DEPTH):
                if dbg and l >= dbg.get("nl", DEPTH):
                    for s_ in order:
                        next_slab(l, s_)
                    continue
                layer(l, sq)
            if not (dbg and not dbg.get("final", True)):
                norm(lambda k: xall[:, k, :], lambda k: xTc[k], lambda k: vcol("finw", k), lambda k: 0.0, [V])
            P.dma("sp", "xout", lambda e, sq=sq, ti=ti: [e.dma_start(out=out[sq, :, :, ti * TT:(ti + 1) * TT], in_=xall)],
                  reads=xTc)
    P.finish()
    print("ops:", len(P.ops), "engine signal counts:", P.counts, "sems:", P.nsem)
    return nc


N_CORES = 8


def kernel(x, c, ada_w, ada_b, mix_norm_w, in_w, in_b, mlstm_conv_w, mlstm_conv_b,
           mlstm_norm_w, hgrn_lower_bounds, hgrn_norm_w, out_w, ffn_norm_w, ffn_up_w,
           ffn_conv_w, ffn_conv_b, ffn_down_w, final_norm_w):
    inp = dict(x=x, c=c, ada_w=ada_w, ada_b=ada_b, mix_norm_w=mix_norm_w, in_w=in_w, in_b=in_b,
               mlstm_conv_w=mlstm_conv_w, mlstm_conv_b=mlstm_conv_b, mlstm_norm_w=mlstm_norm_w,
               hgrn_lower_bounds=hgrn_lower_bounds, hgrn_norm_w=hgrn_norm_w, out_w=out_w,
               ffn_norm_w=ffn_norm_w, ffn_up_w=ffn_up_w, ffn_conv_w=ffn_conv_w, ffn_conv_b=ffn_conv_b,
               ffn_down_w=ffn_down_w, final_norm_w=final_norm_w)
    inp = {k: np.asarray(v, dtype=np.float32) for k, v in inp.items()}
    B, S, _ = inp["x"].shape
    assert B % N_CORES == 0
    nseq = B // N_CORES
    shared = prep_shared(inp)
    nc = build_nc(nseq, S)
    in_maps = []
    for i in range(N_CORES):
        core = prep_core(inp["x"][i * nseq:(i + 1) * nseq], inp["c"][i * nseq:(i + 1) * nseq])
        in_maps.append({**shared, **core})
    res = run_bass_kernel_spmd(nc, in_maps, core_ids=list(range(N_CORES)))
    outs = [unprep_out(np.asarray(r["oT"])) for r in res.results]
    return np.concatenate(outs, axis=0).astype(np.float32)
```
